# Optimizing a Trainium2 kernel written in Bass

```python
import math
import jax
import jax.numpy as jnp
from jax import lax
import numpy as np

D_MODEL = 1024
BATCH = 8
SEQ = 4096
DEPTH = 2

GRID_W = 64
CTX_LEN = 256
N_BRANCH = 4
BRANCH_W = 512
Q_BLOCK = 128
ROPE_THETA = 10000.0
NEG_INF = -1e30
LN_EPS = 1e-5
RMS_EPS = 1e-6

MLA_HEADS = 8
MLA_Q_RANK = 256
MLA_KV_RANK = 128
MLA_NOPE = 64
MLA_ROPE = 32
MLA_V = 64
MLA_SCALE = (MLA_NOPE + MLA_ROPE) ** -0.5

RWKV_HEADS = 8
RWKV_HEAD = 64
RWKV_W = RWKV_HEADS * RWKV_HEAD
RWKV_W_LORA = 64
RWKV_A_LORA = 64
RWKV_G_LORA = 128
RWKV_GN_EPS = 64e-5

GQA_HEADS = 8
GQA_KV_HEADS = 2
GQA_HEAD = 64
GQA_SCALE = GQA_HEAD ** -0.5

WIN_HEADS = 8
WIN_KV_HEADS = 2
WIN_HEAD = 64
WINDOW = 128
WIN_SCALE = WIN_HEAD ** -0.5

PEER_HEADS = 8
PEER_N_KEYS = 128
PEER_EXPERTS = PEER_N_KEYS * PEER_N_KEYS
PEER_TOPK = 16
PEER_DQ = 128
PEER_BLOCK = 128

DEEPNORM_ALPHA = (2 * DEPTH) ** 0.25
DEEPNORM_BETA = (8 * DEPTH) ** -0.25

MLA_IN = MLA_Q_RANK + MLA_KV_RANK + MLA_ROPE
RWKV_IN = 3 * RWKV_W + 2 * RWKV_W_LORA + 2 * RWKV_A_LORA + RWKV_G_LORA
GQA_IN = (GQA_HEADS + 2 * GQA_KV_HEADS) * GQA_HEAD
WIN_IN = (WIN_HEADS + 2 * WIN_KV_HEADS) * WIN_HEAD
GATE_IN = N_BRANCH * D_MODEL
GROUP_WIDTHS = (MLA_IN, RWKV_IN, GQA_IN, WIN_IN, GATE_IN)
IN_WIDTH = MLA_IN + RWKV_IN + GQA_IN + WIN_IN + GATE_IN

kernel_name = "hybrid_mla_rwkv7_gqa_swa_peer_dit"


def split_last(x, widths):
    out, start = [], 0
    for w in widths:
        out.append(x[..., start:start + w])
        start += w
    return out


def rms_norm(x, g):
    xf = x.astype(jnp.float32)
    y = xf * lax.rsqrt(jnp.mean(xf * xf, axis=-1, keepdims=True) + RMS_EPS) * g.astype(jnp.float32)
    return y.astype(x.dtype)


def layer_norm(x, g, b):
    xf = x.astype(jnp.float32)
    mu = jnp.mean(xf, axis=-1, keepdims=True)
    var = jnp.mean(jnp.square(xf - mu), axis=-1, keepdims=True)
    y = (xf - mu) * lax.rsqrt(var + LN_EPS) * g.astype(jnp.float32) + b.astype(jnp.float32)
    return y.astype(x.dtype)


def axial_rope_tables(rows, rot_dim):
    r_idx = jnp.repeat(jnp.arange(rows), GRID_W).astype(jnp.float32)
    c_idx = jnp.tile(jnp.arange(GRID_W), rows).astype(jnp.float32)
    n = rot_dim // 4
    inv = ROPE_THETA ** (-jnp.arange(n, dtype=jnp.float32) / n)
    ang = jnp.concatenate([r_idx[:, None] * inv, c_idx[:, None] * inv], axis=-1)
    return jnp.cos(ang), jnp.sin(ang)


def apply_rope(x, cos, sin):
    xf = x.astype(jnp.float32).reshape(x.shape[:-1] + (x.shape[-1] // 2, 2))
    x1, x2 = xf[..., 0], xf[..., 1]
    cs, sn = cos[None, :, None, :], sin[None, :, None, :]
    out = jnp.stack([x1 * cs - x2 * sn, x1 * sn + x2 * cs], axis=-1).reshape(x.shape)
    return out.astype(x.dtype)


def dense_attention(q, k, v, scale, sink=None):
    B, S, H, dk = q.shape
    Hk = k.shape[2]
    G = H // Hk
    dv = v.shape[-1]
    nb = S // Q_BLOCK
    qb = q.reshape(B, nb, Q_BLOCK, Hk, G, dk).swapaxes(0, 1)

    def one(qi):
        s = jnp.einsum('bqkgd,blkd->bkgql', qi, k).astype(jnp.float32) * scale
        if sink is not None:
            sk = jnp.broadcast_to(sink.astype(jnp.float32).reshape(Hk, G, 1, 1), s.shape[:-1] + (1,))
            p = jax.nn.softmax(jnp.concatenate([s, sk], axis=-1), axis=-1)[..., :-1]
        else:
            p = jax.nn.softmax(s, axis=-1)
        return jnp.einsum('bkgql,blkd->bqkgd', p.astype(v.dtype), v)

    o = lax.map(one, qb)
    return o.swapaxes(0, 1).reshape(B, S, H, dv)


def window_attention(q, k, v, k_ctx, v_ctx, sink, scale):
    B, S, H, d = q.shape
    Hk = k.shape[2]
    G = H // Hk
    C = k_ctx.shape[1]
    nb = S // WINDOW
    pad = ((0, 0), (WINDOW, WINDOW), (0, 0), (0, 0))
    kp, vp = jnp.pad(k, pad), jnp.pad(v, pad)
    k_win = jnp.concatenate([kp[:, i * WINDOW:i * WINDOW + S].reshape(B, nb, WINDOW, Hk, d) for i in range(3)], axis=2)
    v_win = jnp.concatenate([vp[:, i * WINDOW:i * WINDOW + S].reshape(B, nb, WINDOW, Hk, d) for i in range(3)], axis=2)
    q_pos = jnp.arange(S).reshape(nb, WINDOW)
    k_pos = q_pos[:, :1] - WINDOW + jnp.arange(3 * WINDOW)[None, :]
    valid = ((jnp.abs(q_pos[:, :, None] - k_pos[:, None, :]) <= WINDOW)
             & (k_pos[:, None, :] >= 0) & (k_pos[:, None, :] < S))
    qb = q.reshape(B, nb, WINDOW, Hk, G, d).swapaxes(0, 1)
    sink_b = sink.astype(jnp.float32).reshape(Hk, G, 1, 1)
    n_loc = 3 * WINDOW

    def one(args):
        qi, ki, vi, mi = args
        s_loc = jnp.einsum('bqkgd,blkd->bkgql', qi, ki).astype(jnp.float32) * scale
        s_loc = jnp.where(mi[None, None, None], s_loc, NEG_INF)
        s_ctx = jnp.einsum('bqkgd,blkd->bkgql', qi, k_ctx).astype(jnp.float32) * scale
        s_snk = jnp.broadcast_to(sink_b, s_loc.shape[:-1] + (1,))
        p = jax.nn.softmax(jnp.concatenate([s_loc, s_ctx, s_snk], axis=-1), axis=-1).astype(v.dtype)
        return (jnp.einsum('bkgql,blkd->bqkgd', p[..., :n_loc], vi)
                + jnp.einsum('bkgql,blkd->bqkgd', p[..., n_loc:n_loc + C], v_ctx))

    o = lax.map(one, (qb, k_win.swapaxes(0, 1), v_win.swapaxes(0, 1), valid))
    return o.swapaxes(0, 1).reshape(B, S, H, d)


def mla_qkv(p, q_norm, kv_norm, w_uq, w_ukv, rope):
    B, L, _ = p.shape
    dq, dkv, kr = split_last(p, (MLA_Q_RANK, MLA_KV_RANK, MLA_ROPE))
    q = (rms_norm(dq, q_norm) @ w_uq).reshape(B, L, MLA_HEADS, MLA_NOPE + MLA_ROPE)
    kv = (rms_norm(dkv, kv_norm) @ w_ukv).reshape(B, L, MLA_HEADS, MLA_NOPE + MLA_V)
    q_nope, q_rope = q[..., :MLA_NOPE], q[..., MLA_NOPE:]
    k_nope, v = kv[..., :MLA_NOPE], kv[..., MLA_NOPE:]
    k_rope = kr.reshape(B, L, 1, MLA_ROPE)
    if rope is not None:
        q_rope = apply_rope(q_rope, rope[0], rope[1])
        k_rope = apply_rope(k_rope, rope[0], rope[1])
    q = jnp.concatenate([q_nope, q_rope], axis=-1)
    k = jnp.concatenate([k_nope, jnp.broadcast_to(k_rope, (B, L, MLA_HEADS, MLA_ROPE))], axis=-1)
    return q, k, v


def gqa_qkv(p, n_heads, n_kv, hd):
    B, L, _ = p.shape
    q, k, v = split_last(p, (n_heads * hd, n_kv * hd, n_kv * hd))
    return q.reshape(B, L, n_heads, hd), k.reshape(B, L, n_kv, hd), v.reshape(B, L, n_kv, hd)


def rwkv7_features(p, mu, w0, w2, a0, a2, g2, k_k, k_a):
    B, L, _ = p.shape
    prev = jnp.pad(p[:, :-1], ((0, 0), (1, 0), (0, 0)))
    nxt = jnp.pad(p[:, 1:], ((0, 0), (0, 1), (0, 0)))
    p = (p + mu[0] * (prev - p) + mu[1] * (nxt - p)).astype(jnp.float32)
    r, k, v, wf, wb, af, ab, gi = split_last(
        p, (RWKV_W, RWKV_W, RWKV_W, RWKV_W_LORA, RWKV_W_LORA, RWKV_A_LORA, RWKV_A_LORA, RWKV_G_LORA))

    def heads(t):
        return t.reshape(B, L, RWKV_HEADS, RWKV_HEAD)

    kk = heads(k * k_k)
    kk = kk / jnp.maximum(jnp.sqrt(jnp.sum(kk * kk, axis=-1, keepdims=True)), 1e-12)
    decay, kd, ad = [], [], []
    for d, (w_in_d, a_in_d) in enumerate(((wf, af), (wb, ab))):
        lw = -jax.nn.softplus(-(w0[d] + jnp.tanh(w_in_d) @ w2[d])) - 0.5
        a = jax.nn.sigmoid(a0[d] + a_in_d @ a2[d])
        decay.append(heads(jnp.exp(-jnp.exp(lw))))
        ad.append(heads(a))
        kd.append(heads(k * (1.0 + (a - 1.0) * k_a)))
    g = jax.nn.sigmoid(gi) @ g2
    return {'r': heads(r), 'v': heads(v), 'kk': kk, 'g': g, 'decay': decay, 'k': kd, 'a': ad}


def wkv7_scan(state0, f, d, reverse):
    xs = tuple(t.swapaxes(0, 1) for t in (f['r'], f['decay'][d], f['k'][d], f['v'], f['kk'], f['a'][d]))

    def step(S, inp):
        r, w, k, v, kk, a = inp
        sa = jnp.einsum('bhij,bhj->bhi', S, kk)
        S = S * w[:, :, None, :] - sa[..., None] * (kk * a)[:, :, None, :] + v[..., None] * k[:, :, None, :]
        return S, jnp.einsum('bhij,bhj->bhi', S, r)

    S, ys = lax.scan(step, state0, xs, reverse=reverse)
    return S, ys.swapaxes(0, 1)


def rwkv7_output(f, o_f, o_b, r_k, ln_g, ln_b, dtype):
    o = o_f + o_b
    B, L = o.shape[0], o.shape[1]
    mu = jnp.mean(o, axis=-1, keepdims=True)
    var = jnp.mean(jnp.square(o - mu), axis=-1, keepdims=True)
    o = ((o - mu) * lax.rsqrt(var + RWKV_GN_EPS) * ln_g.reshape(RWKV_HEADS, RWKV_HEAD)
         + ln_b.reshape(RWKV_HEADS, RWKV_HEAD))
    bonus = jnp.sum(f['r'] * (f['k'][0] + f['k'][1]) * r_k, axis=-1, keepdims=True) * f['v']
    y = (o + bonus).reshape(B, L, RWKV_W) * f['g']
    return y.astype(dtype)


def merge_heads(t):
    return t.reshape(t.shape[0], t.shape[1], -1)


def merge_branches(ys, gate_pre, w_branch, w_out):
    B, L, _ = gate_pre.shape
    gates = jax.nn.sigmoid(gate_pre.astype(jnp.float32)).astype(gate_pre.dtype).reshape(B, L, N_BRANCH, D_MODEL)
    acc = gates[:, :, 0] * (ys[0] @ w_branch[0])
    for i in range(1, N_BRANCH):
        acc = acc + gates[:, :, i] * (ys[i] @ w_branch[i])
    return acc @ w_out


def peer_ffn(h, wq, keys, u_tab, v_tab):
    B, L, D = h.shape
    q = (h @ wq).reshape(B, L, PEER_HEADS, 2, PEER_DQ // 2).astype(jnp.float32)
    s1 = jnp.einsum('blhd,hkd->blhk', q[..., 0, :], keys[:, 0].astype(jnp.float32))
    s2 = jnp.einsum('blhd,hkd->blhk', q[..., 1, :], keys[:, 1].astype(jnp.float32))
    v1, i1 = lax.top_k(s1, PEER_TOPK)
    v2, i2 = lax.top_k(s2, PEER_TOPK)
    cand = (v1[..., :, None] + v2[..., None, :]).reshape(B, L, PEER_HEADS, PEER_TOPK * PEER_TOPK)
    cidx = (i1[..., :, None] * PEER_N_KEYS + i2[..., None, :]).reshape(B, L, PEER_HEADS, PEER_TOPK * PEER_TOPK)
    best, pos = lax.top_k(cand, PEER_TOPK)
    idx = jnp.take_along_axis(cidx, pos, axis=-1)
    wgt = jax.nn.softmax(best, axis=-1)
    E = PEER_HEADS * PEER_TOPK
    nblk = (B * L) // PEER_BLOCK
    hb = h.reshape(nblk, PEER_BLOCK, D)
    ib = idx.reshape(nblk, PEER_BLOCK, E)
    gb = wgt.reshape(nblk, PEER_BLOCK, E).astype(h.dtype)

    def one(args):
        ht, it, gt = args
        act = jax.nn.gelu(jnp.einsum('td,ted->te', ht, jnp.take(u_tab, it, axis=0)), approximate=False)
        return jnp.einsum('te,ted->td', act * gt, jnp.take(v_tab, it, axis=0))

    return lax.map(one, (hb, ib, gb)).reshape(B, L, D)


def setup_inputs(seed: int = 0) -> dict:
    key = jax.random.key(seed)
    ks = iter(jax.random.split(key, 64))
    L, D = DEPTH, D_MODEL

    def nrm(shape, scale):
        return scale * jax.random.normal(next(ks), shape, jnp.float32)

    def gain(shape):
        return 1.0 + nrm(shape, 0.02)

    return {
        'x': nrm((BATCH, SEQ, D), 1.0),
        'c': nrm((BATCH, D), 1.0),
        'ctx': nrm((BATCH, CTX_LEN, D), 1.0),
        'c_ctx': nrm((D,), 1.0),
        'ada_w': nrm((L, D, 6 * D), 0.5 * D ** -0.5),
        'ada_b': nrm((L, 6 * D), 0.02),
        'w_in': nrm((L, D, IN_WIDTH), D ** -0.5),
        'mla_q_norm': gain((L, MLA_Q_RANK)),
        'mla_kv_norm': gain((L, MLA_KV_RANK)),
        'mla_w_uq': nrm((L, MLA_Q_RANK, MLA_HEADS * (MLA_NOPE + MLA_ROPE)), MLA_Q_RANK ** -0.5),
        'mla_w_ukv': nrm((L, MLA_KV_RANK, MLA_HEADS * (MLA_NOPE + MLA_V)), MLA_KV_RANK ** -0.5),
        'rwkv_mu': jax.random.uniform(next(ks), (L, 2, RWKV_IN), jnp.float32, 0.0, 0.5),
        'rwkv_w0': jax.random.uniform(next(ks), (L, 2, RWKV_W), jnp.float32, -6.0, 1.0),
        'rwkv_w2': nrm((L, 2, RWKV_W_LORA, RWKV_W), 0.1 * RWKV_W_LORA ** -0.5),
        'rwkv_a0': nrm((L, 2, RWKV_W), 0.1),
        'rwkv_a2': nrm((L, 2, RWKV_A_LORA, RWKV_W), RWKV_A_LORA ** -0.5),
        'rwkv_g2': nrm((L, RWKV_G_LORA, RWKV_W), RWKV_G_LORA ** -0.5),
        'rwkv_k_k': 0.85 + nrm((L, RWKV_W), 0.02),
        'rwkv_k_a': gain((L, RWKV_W)),
        'rwkv_r_k': nrm((L, RWKV_HEADS, RWKV_HEAD), 0.1),
        'rwkv_ln_g': gain((L, RWKV_W)),
        'rwkv_ln_b': nrm((L, RWKV_W), 0.02),
        'gqa_q_norm': gain((L, GQA_HEAD)),
        'gqa_k_norm': gain((L, GQA_HEAD)),
        'win_sink': nrm((L, WIN_HEADS), 1.0),
        'w_branch': nrm((L, N_BRANCH, BRANCH_W, D), DEEPNORM_BETA * BRANCH_W ** -0.5),
        'w_out': nrm((L, D, D), DEEPNORM_BETA * D ** -0.5),
        'ln1_g': gain((L, D)),
        'ln1_b': nrm((L, D), 0.02),
        'peer_wq': nrm((L, D, PEER_HEADS * PEER_DQ), D ** -0.5),
        'peer_keys': nrm((L, PEER_HEADS, 2, PEER_N_KEYS, PEER_DQ // 2), (PEER_DQ // 2) ** -0.5),
        'peer_u': nrm((L, PEER_EXPERTS, D), D ** -0.5),
        'peer_v': nrm((L, PEER_EXPERTS, D), DEEPNORM_BETA),
        'ln2_g': gain((L, D)),
        'ln2_b': nrm((L, D), 0.02),
    }


def reference(x, c, ctx, c_ctx, ada_w, ada_b, w_in, mla_q_norm, mla_kv_norm, mla_w_uq, mla_w_ukv,
              rwkv_mu, rwkv_w0, rwkv_w2, rwkv_a0, rwkv_a2, rwkv_g2, rwkv_k_k, rwkv_k_a, rwkv_r_k,
              rwkv_ln_g, rwkv_ln_b, gqa_q_norm, gqa_k_norm, win_sink, w_branch, w_out, ln1_g, ln1_b,
              peer_wq, peer_keys, peer_u, peer_v, ln2_g, ln2_b):
    seq_len = x.shape[1]
    rows = seq_len // GRID_W
    rope_mla = axial_rope_tables(rows, MLA_ROPE)
    rope_head = axial_rope_tables(rows, GQA_HEAD)
    for l in range(DEPTH):
        mod_lat = jax.nn.silu(c) @ ada_w[l] + ada_b[l]
        mod_ctx = jax.nn.silu(c_ctx) @ ada_w[l] + ada_b[l]
        sh1, sc1, gt1, sh2, sc2, gt2 = jnp.split(mod_lat[:, None, :], 6, axis=-1)
        csh1, csc1, cgt1, csh2, csc2, cgt2 = jnp.split(mod_ctx, 6, axis=-1)

        p_lat = (x * (1 + sc1) + sh1) @ w_in[l]
        p_ctx = (ctx * (1 + csc1) + csh1) @ w_in[l]
        mla_lat, rwkv_lat, gqa_lat, win_lat, gate_lat = split_last(p_lat, GROUP_WIDTHS)
        mla_ctx, rwkv_ctx, gqa_ctx, win_ctx, gate_ctx = split_last(p_ctx, GROUP_WIDTHS)

        qa, ka, va = mla_qkv(mla_lat, mla_q_norm[l], mla_kv_norm[l], mla_w_uq[l], mla_w_ukv[l], rope_mla)
        qa_c, ka_c, va_c = mla_qkv(mla_ctx, mla_q_norm[l], mla_kv_norm[l], mla_w_uq[l], mla_w_ukv[l], None)
        ya = dense_attention(qa, jnp.concatenate([ka, ka_c], axis=1), jnp.concatenate([va, va_c], axis=1), MLA_SCALE)

        fb = rwkv7_features(rwkv_lat, rwkv_mu[l], rwkv_w0[l], rwkv_w2[l], rwkv_a0[l], rwkv_a2[l],
                            rwkv_g2[l], rwkv_k_k[l], rwkv_k_a[l])
        fb_c = rwkv7_features(rwkv_ctx, rwkv_mu[l], rwkv_w0[l], rwkv_w2[l], rwkv_a0[l], rwkv_a2[l],
                              rwkv_g2[l], rwkv_k_k[l], rwkv_k_a[l])
        s0 = jnp.zeros((ctx.shape[0], RWKV_HEADS, RWKV_HEAD, RWKV_HEAD), jnp.float32)
        sf_c, of_c = wkv7_scan(s0, fb_c, 0, False)
        sb_c, ob_c = wkv7_scan(s0, fb_c, 1, True)
        _, of = wkv7_scan(sf_c, fb, 0, False)
        _, ob = wkv7_scan(sb_c, fb, 1, True)
        yb = rwkv7_output(fb, of, ob, rwkv_r_k[l], rwkv_ln_g[l], rwkv_ln_b[l], x.dtype)

        qc, kc, vc = gqa_qkv(gqa_lat, GQA_HEADS, GQA_KV_HEADS, GQA_HEAD)
        qc_c, kc_c, vc_c = gqa_qkv(gqa_ctx, GQA_HEADS, GQA_KV_HEADS, GQA_HEAD)
        qc = apply_rope(rms_norm(qc, gqa_q_norm[l]), rope_head[0], rope_head[1])
        kc = apply_rope(rms_norm(kc, gqa_k_norm[l]), rope_head[0], rope_head[1])
        qc_c = rms_norm(qc_c, gqa_q_norm[l])
        kc_c = rms_norm(kc_c, gqa_k_norm[l])
        yc = dense_attention(qc, jnp.concatenate([kc, kc_c], axis=1), jnp.concatenate([vc, vc_c], axis=1), GQA_SCALE)

        qd, kd, vd = gqa_qkv(win_lat, WIN_HEADS, WIN_KV_HEADS, WIN_HEAD)
        qd_c, kd_c, vd_c = gqa_qkv(win_ctx, WIN_HEADS, WIN_KV_HEADS, WIN_HEAD)
        qd = apply_rope(qd, rope_head[0], rope_head[1])
        kd = apply_rope(kd, rope_head[0], rope_head[1])
        yd = window_attention(qd, kd, vd, kd_c, vd_c, win_sink[l], WIN_SCALE)

        mix = merge_branches((merge_heads(ya), yb, merge_heads(yc), merge_heads(yd)), gate_lat, w_branch[l], w_out[l])
        x_mid = layer_norm(DEEPNORM_ALPHA * x + gt1 * mix, ln1_g[l], ln1_b[l])
        ffn = peer_ffn(x_mid * (1 + sc2) + sh2, peer_wq[l], peer_keys[l], peer_u[l], peer_v[l])
        x_new = layer_norm(DEEPNORM_ALPHA * x_mid + gt2 * ffn, ln2_g[l], ln2_b[l])

        if l < DEPTH - 1:
            ya_c = dense_attention(qa_c, ka_c, va_c, MLA_SCALE)
            yb_c = rwkv7_output(fb_c, of_c, ob_c, rwkv_r_k[l], rwkv_ln_g[l], rwkv_ln_b[l], ctx.dtype)
            yc_c = dense_attention(qc_c, kc_c, vc_c, GQA_SCALE)
            yd_c = dense_attention(qd_c, kd_c, vd_c, WIN_SCALE, sink=win_sink[l])
            mix_c = merge_branches((merge_heads(ya_c), yb_c, merge_heads(yc_c), merge_heads(yd_c)),
                                   gate_ctx, w_branch[l], w_out[l])
            ctx_mid = layer_norm(DEEPNORM_ALPHA * ctx + cgt1 * mix_c, ln1_g[l], ln1_b[l])
            ffn_c = peer_ffn(ctx_mid * (1 + csc2) + csh2, peer_wq[l], peer_keys[l], peer_u[l], peer_v[l])
            ctx = layer_norm(DEEPNORM_ALPHA * ctx_mid + cgt2 * ffn_c, ln2_g[l], ln2_b[l])
        x = x_new
    return x
```

```python
import math
import numpy as np
import concourse.bass as bass
import concourse.mybir as mybir
from concourse.bass_utils import run_bass_kernel_spmd

F32 = mybir.dt.float32
I32 = mybir.dt.int32
U32 = mybir.dt.uint32
AF = mybir.ActivationFunctionType
ALU = mybir.AluOpType
AX = mybir.AxisListType

D = 1024
SEQ = 4096
CTX = 256
TOK = SEQ + CTX
NT = TOK // 128
DEPTH = 2
IN_W = 7968
OFF_MLA, OFF_RWKV, OFF_GQA, OFF_WIN, OFF_GATE = 0, 416, 2336, 3104, 3872
ALPHA = (2 * DEPTH) ** 0.25
MLA_SCALE = 96 ** -0.5
CHUNKS = [(0, 256)] + [(256 + 512 * i, 512) for i in range(8)]


class Prog:
    ENG = ('pe', 'act', 'dve', 'pool', 'sp')
    NDMA = 8

    def __init__(self, nc):
        self.nc = nc
        self.engobj = {'pe': nc.tensor, 'act': nc.scalar, 'dve': nc.vector, 'pool': nc.gpsimd, 'sp': nc.sync}
        self.sems, self.cnt, self.waited, self.lastw, self.readers = {}, {}, {}, {}, {}
        self.dma_n = {'sp': 0, 'pool': 0}
        self._stack = []
        self.nops = 0
        for e in self.ENG:
            self._mksem('done_' + e)
        for q in self.dma_n:
            for k in range(self.NDMA):
                self._mksem('dma_%s_%d' % (q, k))

    def _mksem(self, name):
        cm = self.nc.semaphore(name)
        self.sems[name] = cm.__enter__()
        self._stack.append(cm)
        self.cnt[name] = 0

    def _need(self, eng, tok, waits):
        if tok is None:
            return
        sname, val = tok
        if eng == 'pe' and sname == 'done_pe':
            return
        key = (eng, sname)
        if self.waited.get(key, 0) >= val:
            return
        self.waited[key] = val
        waits.append((sname, val))

    def _deps(self, eng, reads, writes):
        waits = []
        for b in reads:
            self._need(eng, self.lastw.get(b), waits)
        for b in writes:
            self._need(eng, self.lastw.get(b), waits)
            for t in self.readers.get(b, ()):
                self._need(eng, t, waits)
        return waits

    def _commit(self, tok, reads, writes):
        for b in reads:
            self.readers.setdefault(b, []).append(tok)
        for b in writes:
            self.lastw[b] = tok
            self.readers[b] = []

    def _emit(self, eng, waits, fn, inc):
        e = self.engobj[eng]
        for sname, val in waits:
            e.wait_ge(self.sems[sname], val)
        if fn is not None:
            fn(e).then_inc(self.sems[inc[0]], inc[1])
        self.nops += 1

    def op(self, eng, fn, reads=(), writes=()):
        waits = self._deps(eng, reads, writes)
        sname = 'done_' + eng
        self.cnt[sname] += 1
        tok = (sname, self.cnt[sname])
        self._emit(eng, waits, fn, (sname, 1))
        self._commit(tok, reads, writes)

    def dma(self, fn, reads=(), writes=(), q='sp'):
        waits = self._deps(q, reads, writes)
        n = self.dma_n[q]
        self.dma_n[q] += 1
        sname = 'dma_%s_%d' % (q, n % self.NDMA)
        if self.cnt[sname]:
            self._need(q, (sname, self.cnt[sname]), waits)
        self.cnt[sname] += 16
        tok = (sname, self.cnt[sname])
        self._emit(q, waits, fn, (sname, 16))
        self._commit(tok, reads, writes)

    def barrier(self, engs=None):
        for e in (engs or self.ENG):
            w = []
            for sname, c in self.cnt.items():
                if c:
                    self._need(e, (sname, c), w)
            self._emit(e, w, None, None)
        self.lastw.clear()
        self.readers.clear()


class KB:
    def __init__(self, dbg=()):
        self.nc = bass.Bass("TRN2", target_bir_lowering=False)
        self.p = Prog(self.nc)
        self.dbg = set(dbg)
        self.scopes = []
        self.uid = 0
        self.ins = {}

    def inp(self, name, shape, dt=F32):
        a = self.nc.dram_tensor(name, list(shape), dt, kind="ExternalInput").ap()
        self.ins[name] = a
        return a

    def scratch(self, name, shape, dt=F32, out=False):
        kind = "ExternalOutput" if (out or name in self.dbg) else "Internal"
        return self.nc.dram_tensor(name, list(shape), dt, kind=kind).ap()

    def begin(self):
        self.scopes.append([])

    def end(self):
        self.p.barrier()
        for cm in reversed(self.scopes.pop()):
            cm.__exit__(None, None, None)

    def sb(self, name, shape, dt=F32):
        self.uid += 1
        cm = self.nc.sbuf_tensor("%s_%d" % (name, self.uid), list(shape), dt)
        t = cm.__enter__()
        self.scopes[-1].append(cm)
        return t

    def ps(self, name, shape, dt=F32):
        self.uid += 1
        cm = self.nc.psum_tensor("%s_%d" % (name, self.uid), list(shape), dt)
        t = cm.__enter__()
        self.scopes[-1].append(cm)
        return t


def build(dbg=(), stop_after=None, nlayers=DEPTH, with_rwkv=True, rw_only=False, scan_steps=None, stages=None, rw_parts=None, peer_tiles=None, peer_slots=128, peer_upto=None):
    kb = KB(dbg)
    nc, p = kb.nc, kb.p
    op, dma = p.op, p.dma
    noop = lambda *a, **k: None

    def sub7(k):
        nonlocal op, dma
        if (peer_upto is None or k <= peer_upto) and (stages is None or 7 in stages):
            op, dma = p.op, p.dma
        else:
            op, dma = noop, noop

    def stage_on(n):
        nonlocal op, dma
        if stages is None or n in stages:
            op, dma = p.op, p.dma
        else:
            op, dma = noop, noop
    xin = kb.inp("xin", [TOK, D])
    cc = kb.inp("cc", [2, D])
    ada_w = kb.inp("ada_w", [DEPTH, D, 6 * D])
    ada_b = kb.inp("ada_b", [DEPTH, 6 * D])
    w_in = kb.inp("w_in", [DEPTH, D, IN_W])
    mla_q_norm = kb.inp("mla_q_norm", [DEPTH, 256])
    mla_kv_norm = kb.inp("mla_kv_norm", [DEPTH, 128])
    mla_w_uq = kb.inp("mla_w_uq", [DEPTH, 256, 768])
    mla_w_ukv = kb.inp("mla_w_ukv", [DEPTH, 128, 1024])
    gqa_q_norm = kb.inp("gqa_q_norm", [DEPTH, 64])
    gqa_k_norm = kb.inp("gqa_k_norm", [DEPTH, 64])
    win_sink = kb.inp("win_sink", [DEPTH, 8])
    w_branch = kb.inp("w_branch", [DEPTH, 4, 512, D])
    w_out = kb.inp("w_out", [DEPTH, D, D])
    ln1_g = kb.inp("ln1_g", [DEPTH, D]); ln1_b = kb.inp("ln1_b", [DEPTH, D])
    ln2_g = kb.inp("ln2_g", [DEPTH, D]); ln2_b = kb.inp("ln2_b", [DEPTH, D])
    peer_wq = kb.inp("peer_wq", [DEPTH, D, D])
    peer_keys = kb.inp("peer_keys", [DEPTH, 8, 2, 128, 64])
    peer_u = kb.inp("peer_u", [DEPTH, 16384, D])
    peer_v = kb.inp("peer_v", [DEPTH, 16384, D])
    rw = {}
    for nm, shp in [("rwkv_mu", [DEPTH, 2, 1920]), ("rwkv_w0", [DEPTH, 2, 512]), ("rwkv_w2", [DEPTH, 2, 64, 512]),
                    ("rwkv_a0", [DEPTH, 2, 512]), ("rwkv_a2", [DEPTH, 2, 64, 512]), ("rwkv_g2", [DEPTH, 128, 512]),
                    ("rwkv_k_k", [DEPTH, 512]), ("rwkv_k_a", [DEPTH, 512]), ("rwkv_r_k", [DEPTH, 512]),
                    ("rwkv_ln_g", [DEPTH, 512]), ("rwkv_ln_b", [DEPTH, 512])]:
        rw[nm] = kb.inp(nm, shp)
    c_ident = kb.inp("c_ident", [128, 128])
    c_ones = kb.inp("c_ones", [128, 128])
    c_pswap = kb.inp("c_pswap", [128, 128])
    c_bones = kb.inp("c_bones", [128, 128])
    c_sel = kb.inp("c_sel", [2, 256])
    c_cosA = kb.inp("c_cosA", [128, TOK]); c_sinA = kb.inp("c_sinA", [128, TOK])
    c_cosH = kb.inp("c_cosH", [128, TOK]); c_sinH = kb.inp("c_sinH", [128, TOK])
    c_band = kb.inp("c_band", [128, 256])
    c_iota16 = kb.inp("c_iota16", [128, 16])
    c_hmask = kb.inp("c_hmask", [128, 512])
    yout = kb.scratch("yout", [SEQ, D], out=True)
    XS1 = kb.scratch("XS1", [TOK, D])
    XMID = kb.scratch("XMID", [TOK, D])
    PT = kb.scratch("PT", [IN_W, TOK])
    QA = kb.scratch("QA", [8 * 96, TOK]); KA = kb.scratch("KA", [8 * 96, TOK]); VA = kb.scratch("VA", [TOK, 8, 65])
    QC = kb.scratch("QC", [512, TOK]); KC = kb.scratch("KC", [128, TOK]); VC = kb.scratch("VC", [TOK, 2, 65])
    QD = kb.scratch("QD", [512, TOK]); KD = kb.scratch("KD", [128, TOK]); VD = kb.scratch("VD", [TOK, 2, 65])
    YT = [kb.scratch("YT%d" % i, [512, TOK]) for i in range(4)]

    kb.begin()
    ident = kb.sb("ident", [128, 128]); ones = kb.sb("ones", [128, 128]); pswap = kb.sb("pswap", [128, 128])
    bones = kb.sb("bones", [128, 128]); sel = kb.sb("sel", [2, 256])
    for t, src in [(ident, c_ident), (ones, c_ones), (pswap, c_pswap), (bones, c_bones), (sel, c_sel)]:
        dma(lambda e, t=t, src=src: e.dma_start(out=t[:], in_=src))
    p.barrier()

    for l in range(nlayers):
        last = (l == DEPTH - 1)
        XS = xin if l == 0 else XS1
        XN = XS1
        kb.begin()
        modT = kb.sb("modT", [128, 48, 2])
        A1 = kb.sb("A1", [128, 8, 2])
        bc = {nm: [kb.sb("bc_%s%d" % (nm, w), [128, D]) for w in range(2)] for nm in ("gt1", "sh2", "sc2", "gt2")}
        lnp = {nm: kb.sb("ln_" + nm, [128, D]) for nm in ("g1", "b1", "g2", "b2")}
        if rw_only:
            rwkv_stage(kb, l, PT, YT[1], rw, dict(ident=ident, ones=ones, bones=bones, c_hmask=c_hmask), last, scan_steps)
            kb.end()
            break
        stage_on(0)
        kb.begin()
        scT = kb.sb("scT", [128, 8, 2]); modrow = kb.sb("modrow", [2, 6 * D]); bias2 = kb.sb("bias2", [2, 6 * D])
        wb = [kb.sb("adaw%d" % i, [128, 8, 512]) for i in range(2)]
        pm = [kb.ps("pm%d" % i, [128, 512]) for i in range(2)]
        pt = kb.ps("ptm", [128, 96])
        for r in range(2):
            dma(lambda e, r=r: e.dma_start(out=scT[:, :, r], in_=cc[r].rearrange("(k p) -> p k", p=128), allow_slow_non_contiguous=True), writes=['scT'])
        for r in range(2):
            dma(lambda e, r=r: e.dma_start(out=bias2[r:r + 1, :], in_=ada_b[l:l + 1, :]), writes=['bias2'])
        for (t, src) in [(lnp["g1"], ln1_g), (lnp["b1"], ln1_b), (lnp["g2"], ln2_g), (lnp["b2"], ln2_b)]:
            dma(lambda e, t=t, src=src: e.dma_start(out=t[:], in_=src[l:l + 1, :].partition_broadcast(128) if False else src[l, :].partition_broadcast(128)))
        op('act', lambda e: e.activation(scT[:], scT[:], AF.Silu), reads=['scT'], writes=['scT'])
        for g in range(12):
            W = wb[g % 2]; k_ = 'adaw%d' % (g % 2); P_ = pm[g % 2]; pk_ = 'pm%d' % (g % 2)
            dma(lambda e, W=W, g=g: e.dma_start(out=W[:], in_=ada_w[l, :, g * 512:(g + 1) * 512].rearrange("(k p) n -> p k n", p=128)), writes=[k_])
            for k in range(8):
                op('pe', lambda e, W=W, P_=P_, k=k: e.matmul(P_[0:2, :], scT[:, k, :], W[:, k, :], start=(k == 0), stop=(k == 7)),
                   reads=['scT', k_], writes=[pk_])
            op('dve', lambda e, P_=P_, g=g: e.tensor_tensor(out=modrow[:, g * 512:(g + 1) * 512], in0=P_[0:2, :], in1=bias2[:, g * 512:(g + 1) * 512], op=ALU.add),
               reads=[pk_, 'bias2'], writes=['modrow'])
        for c in range(48):
            op('pe', lambda e, c=c: e.transpose(pt[:, 2 * c:2 * c + 2], modrow[0:2, c * 128:(c + 1) * 128], ident[0:2, 0:2]),
               reads=['modrow'], writes=['ptm'])
        op('dve', lambda e: e.tensor_copy(modT[:].rearrange("p c t -> p (c t)"), pt[:]), reads=['ptm'], writes=['modT'])
        op('dve', lambda e: e.tensor_scalar_add(A1[:], modT[:, 8:16, :], 1.0), reads=['modT'], writes=['A1'])
        for nm, ci in (("gt1", 2), ("sh2", 3), ("sc2", 4), ("gt2", 5)):
            for w in range(2):
                for hf in range(2):
                    P_ = pm[hf]; pk_ = 'pm%d' % hf
                    op('pe', lambda e, P_=P_, w=w, ci=ci, hf=hf: e.matmul(P_[:], sel[:, (1 - w) * 0 + w * 128:(w + 1) * 128] if False else sel[:, w * 128:(w + 1) * 128],
                                                                       modrow[:, ci * D + hf * 512: ci * D + (hf + 1) * 512], start=True, stop=True),
                       reads=['modrow'], writes=[pk_])
                    if nm == "sc2":
                        op('act', lambda e, P_=P_, nm=nm, w=w, hf=hf: e.activation(bc[nm][w][:, hf * 512:(hf + 1) * 512], P_[:], AF.Identity, bias=1.0),
                           reads=[pk_], writes=['bc'])
                    else:
                        op('act', lambda e, P_=P_, nm=nm, w=w, hf=hf: e.activation(bc[nm][w][:, hf * 512:(hf + 1) * 512], P_[:], AF.Identity),
                           reads=[pk_], writes=['bc'])
        kb.end()
        if stop_after == 's0':
            break
        stage_on(1)
        kb.begin()
        xmT = kb.sb("xmT", [128, 8, 512])
        xb = [kb.sb("xb%d" % i, [128, D]) for i in range(2)]
        wb = [kb.sb("winb%d" % i, [128, 8, 512]) for i in range(2)]
        ob = [kb.sb("ob%d" % i, [128, 512]) for i in range(3)]
        ptx = [kb.ps("ptx%d" % i, [128, 128]) for i in range(2)]
        pp = [kb.ps("pp%d" % i, [128, 512]) for i in range(3)]
        cnt = 0
        for ci, (n0, n) in enumerate(CHUNKS):
            which = 1 if ci == 0 else 0
            for sub in range(n // 128):
                t = (n0 // 128) + sub
                X = xb[t % 2]; xk = 'xb%d' % (t % 2)
                dma(lambda e, X=X, t=t: e.dma_start(out=X[:], in_=XS[t * 128:(t + 1) * 128, :]), writes=[xk])
                for k in range(8):
                    P_ = ptx[k % 2]; pk_ = 'ptx%d' % (k % 2)
                    op('pe', lambda e, P_=P_, X=X, k=k: e.transpose(P_[:], X[:, k * 128:(k + 1) * 128], ident[:]), reads=[xk], writes=[pk_])
                    op('act', lambda e, P_=P_, k=k, sub=sub, which=which: e.activation(
                        xmT[:, k, sub * 128:(sub + 1) * 128], P_[:], AF.Identity, scale=A1[:, k, which:which + 1], bias=modT[:, k, which:which + 1]),
                       reads=[pk_], writes=['xmT'])
            for g in range(16):
                c0 = g * 512; ncol = min(512, IN_W - c0)
                W = wb[g % 2]; wk_ = 'winb%d' % (g % 2)
                dma(lambda e, W=W, c0=c0, ncol=ncol: e.dma_start(out=W[:, :, 0:ncol], in_=w_in[l, :, c0:c0 + ncol].rearrange("(k p) n -> p k n", p=128)), writes=[wk_])
                for fb in range((ncol + 127) // 128):
                    m = min(128, ncol - fb * 128)
                    P_ = pp[cnt % 3]; pk_ = 'pp%d' % (cnt % 3); O = ob[cnt % 3]; ok_ = 'ob%d' % (cnt % 3)
                    for k in range(8):
                        op('pe', lambda e, P_=P_, W=W, k=k, fb=fb, m=m, n=n: e.matmul(P_[0:m, 0:n], W[:, k, fb * 128:fb * 128 + m], xmT[:, k, 0:n], start=(k == 0), stop=(k == 7)),
                           reads=[wk_, 'xmT'], writes=[pk_])
                    eng = 'act' if cnt % 2 == 0 else 'dve'
                    if eng == 'act':
                        op('act', lambda e, O=O, P_=P_, m=m, n=n: e.copy(O[0:m, 0:n], P_[0:m, 0:n]), reads=[pk_], writes=[ok_])
                    else:
                        op('dve', lambda e, O=O, P_=P_, m=m, n=n: e.tensor_copy(O[0:m, 0:n], P_[0:m, 0:n]), reads=[pk_], writes=[ok_])
                    r0 = c0 + fb * 128
                    dma(lambda e, O=O, r0=r0, m=m, n0=n0, n=n: e.dma_start(out=PT[r0:r0 + m, n0:n0 + n], in_=O[0:m, 0:n]), reads=[ok_])
                    cnt += 1
        kb.end()
        if stop_after == 's1':
            break
        stage_on(2)
        kb.begin()
        wuq = kb.sb("wuq", [128, 2, 768]); wukv = kb.sb("wukv", [128, 1024])
        gq = kb.sb("gq", [128, 2]); gkv = kb.sb("gkv", [128, 1])
        dq = kb.sb("dq", [128, 2, 512]); dkv = kb.sb("dkv", [128, 512]); kr = kb.sb("kr", [128, 512]); krr = kb.sb("krr", [128, 512])
        sq = kb.sb("sq", [128, 3, 512]); rq = kb.sb("rq", [128, 512]); rkv = kb.sb("rkv", [128, 512])
        cosA = kb.sb("cosA", [128, 512]); sinA = kb.sb("sinA", [128, 512])
        t1 = kb.sb("t1", [128, 512]); t2 = kb.sb("t2", [128, 512])
        qh = [kb.sb("qh%d" % i, [128, 512]) for i in range(2)]
        kh = [kb.sb("kh%d" % i, [128, 512]) for i in range(2)]
        va = [kb.sb("va%d" % i, [128, 8, 65]) for i in range(2)]
        rt = kb.sb("rt", [128, 1])
        pss = [kb.ps("pss%d" % i, [128, 512]) for i in range(2)]
        pq = kb.ps("pq", [128, 512]); pk = kb.ps("pk", [128, 512]); py = kb.ps("py", [128, 512])
        pv = kb.ps("pv", [128, 1024]); pr = kb.ps("pr", [128, 8])
        dma(lambda e: e.dma_start(out=wuq[:], in_=mla_w_uq[l].rearrange("(k p) n -> p k n", p=128)), writes=['wuq'])
        dma(lambda e: e.dma_start(out=wukv[:], in_=mla_w_ukv[l]), writes=['wukv'])
        dma(lambda e: e.dma_start(out=gq[:], in_=mla_q_norm[l].rearrange("(k p) -> p k", p=128), allow_slow_non_contiguous=True), writes=['gq'])
        dma(lambda e: e.dma_start(out=gkv[:], in_=mla_kv_norm[l].rearrange("(p o) -> p o", o=1)), writes=['gkv'])
        for i in range(2):
            op('pool', lambda e, i=i: e.memset(va[i][:], 1.0), writes=['va%d' % i])
        hc = 0
        for ci, (n0, n) in enumerate(CHUNKS):
            dma(lambda e, n0=n0, n=n: e.dma_start(out=dq[:, :, 0:n], in_=PT[0:256, n0:n0 + n].rearrange("(k p) n -> p k n", p=128)), writes=['dq'])
            dma(lambda e, n0=n0, n=n: e.dma_start(out=dkv[:, 0:n], in_=PT[256:384, n0:n0 + n]), writes=['dkv'])
            dma(lambda e, n0=n0, n=n: e.dma_start(out=kr[64:96, 0:n], in_=PT[384:416, n0:n0 + n]), writes=['kr'])
            dma(lambda e, n0=n0, n=n: e.dma_start(out=cosA[64:96, 0:n], in_=c_cosA[64:96, n0:n0 + n]), writes=['cosA'])
            dma(lambda e, n0=n0, n=n: e.dma_start(out=sinA[64:96, 0:n], in_=c_sinA[64:96, n0:n0 + n]), writes=['sinA'])
            op('act', lambda e, n=n: e.activation(sq[:, 0:2, 0:n], dq[:, :, 0:n], AF.Square), reads=['dq'], writes=['sq'])
            op('act', lambda e, n=n: e.activation(sq[:, 2, 0:n], dkv[:, 0:n], AF.Square), reads=['dkv'], writes=['sq'])
            for k in range(2):
                op('pe', lambda e, k=k, n=n: e.matmul(pss[0][:, 0:n], ones[:], sq[:, k, 0:n], start=(k == 0), stop=(k == 1)), reads=['sq'], writes=['pss0'])
            op('pe', lambda e, n=n: e.matmul(pss[1][:, 0:n], ones[:], sq[:, 2, 0:n], start=True, stop=True), reads=['sq'], writes=['pss1'])
            op('dve', lambda e, n=n: e.tensor_scalar(rq[:, 0:n], pss[0][:, 0:n], 1.0 / 256, 1e-6, op0=ALU.mult, op1=ALU.add), reads=['pss0'], writes=['rq'])
            op('act', lambda e, n=n: e.activation(rq[:, 0:n], rq[:, 0:n], AF.Sqrt), reads=['rq'], writes=['rq'])
            op('dve', lambda e, n=n: e.reciprocal(rq[:, 0:n], rq[:, 0:n]), reads=['rq'], writes=['rq'])
            op('dve', lambda e, n=n: e.tensor_scalar(rkv[:, 0:n], pss[1][:, 0:n], 1.0 / 128, 1e-6, op0=ALU.mult, op1=ALU.add), reads=['pss1'], writes=['rkv'])
            op('act', lambda e, n=n: e.activation(rkv[:, 0:n], rkv[:, 0:n], AF.Sqrt), reads=['rkv'], writes=['rkv'])
            op('dve', lambda e, n=n: e.reciprocal(rkv[:, 0:n], rkv[:, 0:n]), reads=['rkv'], writes=['rkv'])
            for k in range(2):
                op('act', lambda e, k=k, n=n: e.mul(dq[:, k, 0:n], dq[:, k, 0:n], gq[:, k:k + 1]), reads=['dq', 'gq'], writes=['dq'])
            op('act', lambda e, n=n: e.mul(dkv[:, 0:n], dkv[:, 0:n], gkv[:, 0:1]), reads=['dkv', 'gkv'], writes=['dkv'])
            op('pe', lambda e, n=n: e.matmul(py[64:96, 0:n], pswap[64:96, 64:96], kr[64:96, 0:n], start=True, stop=True), reads=['kr'], writes=['py'])
            op('dve', lambda e, n=n: e.tensor_tensor(out=t1[64:96, 0:n], in0=kr[64:96, 0:n], in1=cosA[64:96, 0:n], op=ALU.mult), reads=['kr', 'cosA'], writes=['t1'])
            op('dve', lambda e, n=n: e.tensor_tensor(out=t2[64:96, 0:n], in0=py[64:96, 0:n], in1=sinA[64:96, 0:n], op=ALU.mult), reads=['py', 'sinA'], writes=['t2'])
            op('dve', lambda e, n=n: e.tensor_tensor(out=krr[64:96, 0:n], in0=t1[64:96, 0:n], in1=t2[64:96, 0:n], op=ALU.add), reads=['t1', 't2'], writes=['krr'])
            for h in range(8):
                Q = qh[hc % 2]; qk_ = 'qh%d' % (hc % 2); K_ = kh[hc % 2]; kk_ = 'kh%d' % (hc % 2)
                hc += 1
                for k in range(2):
                    op('pe', lambda e, k=k, h=h, n=n: e.matmul(pq[0:96, 0:n], wuq[:, k, h * 96:(h + 1) * 96], dq[:, k, 0:n], start=(k == 0), stop=(k == 1)),
                       reads=['wuq', 'dq'], writes=['pq'])
                op('dve', lambda e, Q=Q, n=n: e.scalar_tensor_tensor(out=Q[0:96, 0:n], in0=pq[0:96, 0:n], scalar=MLA_SCALE, in1=rq[0:96, 0:n], op0=ALU.mult, op1=ALU.mult),
                   reads=['pq', 'rq'], writes=[qk_])
                op('pe', lambda e, Q=Q, n=n: e.matmul(py[64:96, 0:n], pswap[64:96, 64:96], Q[64:96, 0:n], start=True, stop=True), reads=[qk_], writes=['py'])
                op('dve', lambda e, Q=Q, n=n: e.tensor_tensor(out=t1[64:96, 0:n], in0=Q[64:96, 0:n], in1=cosA[64:96, 0:n], op=ALU.mult), reads=[qk_, 'cosA'], writes=['t1'])
                op('dve', lambda e, n=n: e.tensor_tensor(out=t2[64:96, 0:n], in0=py[64:96, 0:n], in1=sinA[64:96, 0:n], op=ALU.mult), reads=['py', 'sinA'], writes=['t2'])
                op('dve', lambda e, Q=Q, n=n: e.tensor_tensor(out=Q[64:96, 0:n], in0=t1[64:96, 0:n], in1=t2[64:96, 0:n], op=ALU.add), reads=['t1', 't2'], writes=[qk_])
                dma(lambda e, Q=Q, h=h, n0=n0, n=n: e.dma_start(out=QA[h * 96:(h + 1) * 96, n0:n0 + n], in_=Q[0:96, 0:n]), reads=[qk_])
                op('pe', lambda e, h=h, n=n: e.matmul(pk[0:64, 0:n], wukv[:, h * 128:h * 128 + 64], dkv[:, 0:n], start=True, stop=True), reads=['wukv', 'dkv'], writes=['pk'])
                op('dve', lambda e, K_=K_, n=n: e.tensor_tensor(out=K_[0:64, 0:n], in0=pk[0:64, 0:n], in1=rkv[0:64, 0:n], op=ALU.mult), reads=['pk', 'rkv'], writes=[kk_])
                op('pool', lambda e, K_=K_, n=n: e.tensor_copy(K_[64:96, 0:n], krr[64:96, 0:n]), reads=['krr'], writes=[kk_])
                dma(lambda e, K_=K_, h=h, n0=n0, n=n: e.dma_start(out=KA[h * 96:(h + 1) * 96, n0:n0 + n], in_=K_[0:96, 0:n]), reads=[kk_])
            for sub in range(n // 128):
                t = n0 // 128 + sub
                V_ = va[t % 2]; vk_ = 'va%d' % (t % 2)
                op('pe', lambda e, sub=sub: e.matmul(pr[:, 0:1], sq[:, 2, sub * 128:(sub + 1) * 128], ones[:, 0:1], start=True, stop=True), reads=['sq'], writes=['pr'])
                op('dve', lambda e: e.tensor_scalar(rt[:], pr[:, 0:1], 1.0 / 128, 1e-6, op0=ALU.mult, op1=ALU.add), reads=['pr'], writes=['rt'])
                op('act', lambda e: e.activation(rt[:], rt[:], AF.Sqrt), reads=['rt'], writes=['rt'])
                op('dve', lambda e: e.reciprocal(rt[:], rt[:]), reads=['rt'], writes=['rt'])
                for hf in range(2):
                    op('pe', lambda e, sub=sub, hf=hf: e.matmul(pv[:, hf * 512:(hf + 1) * 512], dkv[:, sub * 128:(sub + 1) * 128], wukv[:, hf * 512:(hf + 1) * 512], start=True, stop=True),
                       reads=['dkv', 'wukv'], writes=['pv'])
                op('act', lambda e, V_=V_: e.activation(V_[:, :, 0:64], pv[:].rearrange("p (h c) -> p h c", c=128)[:, :, 64:128], AF.Copy, scale=rt[:, 0:1]),
                   reads=['pv', 'rt'], writes=[vk_])
                dma(lambda e, V_=V_, t=t: e.dma_start(out=VA[t * 128:(t + 1) * 128, :, :], in_=V_[:]), reads=[vk_])
        kb.end()
        if stop_after == 's2':
            break
        stage_on(3)
        kb.begin()
        cosH = kb.sb("cosH", [128, 512]); sinH = kb.sb("sinH", [128, 512])
        gn = kb.sb("gn", [128, 2])
        X3 = [kb.sb("x3_%d" % i, [128, 512]) for i in range(2)]
        sq3 = kb.sb("sq3", [128, 512]); r3 = kb.sb("r3", [128, 512]); t1 = kb.sb("t1b", [128, 512]); t2 = kb.sb("t2b", [128, 512])
        O3 = [kb.sb("o3_%d" % i, [128, 512]) for i in range(2)]
        vc = [kb.sb("vc%d" % i, [128, 2, 65]) for i in range(2)]
        pss3 = kb.ps("pss3", [128, 512]); py3 = kb.ps("py3", [128, 512]); pt3 = kb.ps("pt3", [128, 128])
        for j, src in enumerate((gqa_q_norm, gqa_k_norm)):
            for hh in range(2):
                dma(lambda e, j=j, src=src, hh=hh: e.dma_start(out=gn[hh * 64:(hh + 1) * 64, j:j + 1], in_=src[l].rearrange("(p o) -> p o", o=1)), writes=['gn'])
        for i in range(2):
            op('pool', lambda e, i=i: e.memset(vc[i][:], 1.0), writes=['vc%d' % i])
        xc = 0
        for (base, norm, Qd, Kd, Vd) in ((OFF_GQA, True, QC, KC, VC), (OFF_WIN, False, QD, KD, VD)):
            for ci, (n0, n) in enumerate(CHUNKS):
                dma(lambda e, n0=n0, n=n: e.dma_start(out=cosH[:, 0:n], in_=c_cosH[:, n0:n0 + n]), writes=['cosH'])
                dma(lambda e, n0=n0, n=n: e.dma_start(out=sinH[:, 0:n], in_=c_sinH[:, n0:n0 + n]), writes=['sinH'])
                for fb in range(5):
                    X = X3[xc % 2]; xk = 'x3_%d' % (xc % 2); O = O3[xc % 2]; ok_ = 'o3_%d' % (xc % 2)
                    xc += 1
                    dma(lambda e, X=X, base=base, fb=fb, n0=n0, n=n: e.dma_start(out=X[:, 0:n], in_=PT[base + fb * 128: base + (fb + 1) * 128, n0:n0 + n]), writes=[xk])
                    if norm:
                        gcol = 0 if fb < 4 else 1
                        op('act', lambda e, X=X, n=n: e.activation(sq3[:, 0:n], X[:, 0:n], AF.Square), reads=[xk], writes=['sq3'])
                        op('pe', lambda e, n=n: e.matmul(pss3[:, 0:n], bones[:], sq3[:, 0:n], start=True, stop=True), reads=['sq3'], writes=['pss3'])
                        op('dve', lambda e, n=n: e.tensor_scalar(r3[:, 0:n], pss3[:, 0:n], 1.0 / 64, 1e-6, op0=ALU.mult, op1=ALU.add), reads=['pss3'], writes=['r3'])
                        op('act', lambda e, n=n: e.activation(r3[:, 0:n], r3[:, 0:n], AF.Sqrt), reads=['r3'], writes=['r3'])
                        op('dve', lambda e, n=n: e.reciprocal(r3[:, 0:n], r3[:, 0:n]), reads=['r3'], writes=['r3'])
                        op('dve', lambda e, X=X, n=n, gcol=gcol: e.scalar_tensor_tensor(out=X[:, 0:n], in0=X[:, 0:n], scalar=gn[:, gcol:gcol + 1], in1=r3[:, 0:n], op0=ALU.mult, op1=ALU.mult),
                           reads=[xk, 'r3', 'gn'], writes=[xk])
                    op('pe', lambda e, X=X, n=n: e.matmul(py3[:, 0:n], pswap[:], X[:, 0:n], start=True, stop=True), reads=[xk], writes=['py3'])
                    op('pool', lambda e, X=X, n=n: e.tensor_tensor(out=t1[:, 0:n], in0=X[:, 0:n], in1=cosH[:, 0:n], op=ALU.mult), reads=[xk, 'cosH'], writes=['t1b'])
                    op('dve', lambda e, n=n: e.tensor_tensor(out=t2[:, 0:n], in0=py3[:, 0:n], in1=sinH[:, 0:n], op=ALU.mult), reads=['py3', 'sinH'], writes=['t2b'])
                    op('dve', lambda e, O=O, n=n: e.tensor_tensor(out=O[:, 0:n], in0=t1[:, 0:n], in1=t2[:, 0:n], op=ALU.add), reads=['t1b', 't2b'], writes=[ok_])
                    if fb < 4:
                        dma(lambda e, O=O, Qd=Qd, fb=fb, n0=n0, n=n: e.dma_start(out=Qd[fb * 128:(fb + 1) * 128, n0:n0 + n], in_=O[:, 0:n]), reads=[ok_])
                    else:
                        dma(lambda e, O=O, Kd=Kd, n0=n0, n=n: e.dma_start(out=Kd[:, n0:n0 + n], in_=O[:, 0:n]), reads=[ok_])
                X = X3[xc % 2]; xk = 'x3_%d' % (xc % 2)
                xc += 1
                dma(lambda e, X=X, base=base, n0=n0, n=n: e.dma_start(out=X[:, 0:n], in_=PT[base + 640: base + 768, n0:n0 + n]), writes=[xk])
                for sub in range(n // 128):
                    t = n0 // 128 + sub
                    V_ = vc[t % 2]; vk_ = 'vc%d' % (t % 2)
                    op('pe', lambda e, X=X, sub=sub: e.transpose(pt3[:], X[:, sub * 128:(sub + 1) * 128], ident[:]), reads=[xk], writes=['pt3'])
                    op('act', lambda e, V_=V_: e.activation(V_[:, :, 0:64], pt3[:].rearrange("p (h c) -> p h c", c=64), AF.Copy), reads=['pt3'], writes=[vk_])
                    dma(lambda e, V_=V_, Vd=Vd, t=t: e.dma_start(out=Vd[t * 128:(t + 1) * 128, :, :], in_=V_[:]), reads=[vk_])
        kb.end()
        if stop_after == 's3':
            break
        stage_on(4)
        qtiles_ctx = [] if last else [0, 1]

        def attention(Qd, Kd, Vd, dq_, nkv, G, scale, mode, Yd, sinkp):
            kb.begin()
            Kh = kb.sb("Kh", [128, TOK]); Vh = kb.sb("Vh", [128, NT, 65])
            Qh = [kb.sb("Qh%d" % i, [128, 512]) for i in range(2)]
            PtAll = kb.sb("PtAll", [128, NT, 256])
            Oall = kb.sb("Oall", [128, NT, 512])
            rec = kb.sb("rec", [128, 4]); den = kb.sb("den", [128, 4])
            band = kb.sb("band", [128, 256]); esink = kb.sb("esink", [128, 8])
            ot = [kb.sb("ot%d" % i, [128, 128]) for i in range(2)]
            psS = [kb.ps("psS%d" % i, [128, 512]) for i in range(3)]
            po = [kb.ps("po%d" % i, [128, 128]) for i in range(2)]
            ptt = [kb.ps("ptt%d" % i, [128, 128]) for i in range(2)]
            dma(lambda e: e.dma_start(out=band[:], in_=c_band), writes=['band'])
            if sinkp:
                dma(lambda e: e.dma_start(out=esink[:], in_=win_sink[l, :].partition_broadcast(128)), writes=['esink'])
                op('act', lambda e: e.activation(esink[:], esink[:], AF.Exp), reads=['esink'], writes=['esink'])
            groups = []
            if qtiles_ctx:
                groups.append(([0, 1], [(0, None), (1, None)], True))
            if mode == 'full':
                for c in range(16):
                    groups.append(([2 + 2 * c, 3 + 2 * c], [(kt, None) for kt in range(NT)], False))
            else:
                for t in range(2, NT):
                    ks = [(0, None), (1, None)]
                    if t - 1 >= 2:
                        ks.append((t - 1, 0))
                    ks.append((t, None))
                    if t + 1 < NT:
                        ks.append((t + 1, 1))
                    groups.append(([t], ks, False))
            cq = 0; cp = 0; ca = 0
            for kvh in range(nkv):
                dma(lambda e, kvh=kvh: e.dma_start(out=Kh[0:dq_, :], in_=Kd[kvh * dq_:(kvh + 1) * dq_, :]), writes=['Kh'])
                dma(lambda e, kvh=kvh: e.dma_start(out=Vh[:], in_=Vd[:, kvh, :].rearrange("(t p) c -> p t c", p=128)), writes=['Vh'])
                for (qts, ks, isctx) in groups:
                    n = 128 * len(qts); q0 = qts[0] * 128
                    for g in range(G):
                        h = kvh * G + g
                        Q = Qh[cq % 2]; qk_ = 'Qh%d' % (cq % 2); cq += 1
                        dma(lambda e, Q=Q, h=h, q0=q0, n=n: e.dma_start(out=Q[0:dq_, 0:n], in_=Qd[h * dq_:(h + 1) * dq_, q0:q0 + n]), writes=[qk_])
                        for ki, (kt, mk) in enumerate(ks):
                            S = psS[cp % 3]; sk_ = 'psS%d' % (cp % 3); cp += 1
                            pk_ = 'PtAll%d' % ki
                            op('pe', lambda e, S=S, Q=Q, kt=kt, n=n: e.matmul(S[:, 0:n], Kh[0:dq_, kt * 128:(kt + 1) * 128], Q[0:dq_, 0:n], start=True, stop=True),
                               reads=['Kh', qk_], writes=[sk_])
                            op('act', lambda e, S=S, ki=ki, n=n: e.activation(PtAll[:, ki, 0:n], S[:, 0:n], AF.Exp, scale=scale), reads=[sk_], writes=[pk_])
                            if mk is not None:
                                op('pool', lambda e, ki=ki, mk=mk: e.tensor_tensor(out=PtAll[:, ki, 0:128], in0=PtAll[:, ki, 0:128], in1=band[:, mk * 128:(mk + 1) * 128], op=ALU.mult),
                                   reads=[pk_, 'band'], writes=[pk_])
                        for sub in range(len(qts)):
                            PO = po[ca % 2]; pok = 'po%d' % (ca % 2); ca += 1
                            for ki, (kt, mk) in enumerate(ks):
                                op('pe', lambda e, PO=PO, sub=sub, kt=kt, ki=ki, nk=len(ks): e.matmul(
                                    PO[:, 0:65], PtAll[:, ki, sub * 128:(sub + 1) * 128], Vh[:, kt, :], start=(ki == 0), stop=(ki == nk - 1)),
                                   reads=['PtAll%d' % ki, 'Vh'], writes=[pok])
                            if sinkp:
                                op('dve', lambda e, PO=PO, h=h: e.tensor_scalar(den[:, 0:1], PO[:, 64:65], esink[:, h:h + 1], None, op0=ALU.add), reads=[pok, 'esink'], writes=['den'])
                                op('dve', lambda e: e.reciprocal(rec[:, 0:1], den[:, 0:1]), reads=['den'], writes=['rec'])
                            else:
                                op('dve', lambda e, PO=PO: e.reciprocal(rec[:, 0:1], PO[:, 64:65]), reads=[pok], writes=['rec'])
                            op('dve', lambda e, PO=PO, t=qts[sub], h=h: e.tensor_scalar(Oall[:, t, h * 64:(h + 1) * 64], PO[:, 0:64], rec[:, 0:1], None, op0=ALU.mult),
                               reads=[pok, 'rec'], writes=['Oall'])
            ct = 0
            for t in (qtiles_ctx + list(range(2, NT))):
                for fb in range(4):
                    P_ = ptt[ct % 2]; pk_ = 'ptt%d' % (ct % 2); O = ot[ct % 2]; ok_ = 'ot%d' % (ct % 2); ct += 1
                    op('pe', lambda e, P_=P_, t=t, fb=fb: e.transpose(P_[:], Oall[:, t, fb * 128:(fb + 1) * 128], ident[:]), reads=['Oall'], writes=[pk_])
                    op('act', lambda e, P_=P_, O=O: e.copy(O[:], P_[:]), reads=[pk_], writes=[ok_])
                    dma(lambda e, O=O, t=t, fb=fb: e.dma_start(out=Yd[fb * 128:(fb + 1) * 128, t * 128:(t + 1) * 128], in_=O[:]), reads=[ok_])
            kb.end()

        attention(QA, KA, VA, 96, 8, 1, 1.0, 'full', YT[0], False)
        attention(QC, KC, VC, 64, 2, 4, 0.125, 'full', YT[2], False)
        attention(QD, KD, VD, 64, 2, 4, 0.125, 'win', YT[3], True)
        if stop_after == 's4':
            break
        if with_rwkv and (stages is None or 5 in stages):
            rwkv_stage(kb, l, PT, YT[1], rw, dict(ident=ident, ones=ones, bones=bones, c_hmask=c_hmask), last, scan_steps, rw_parts)
        elif stages is None or 5 in stages:
            kb.begin()
            z = kb.sb("zz", [128, TOK])
            op('pool', lambda e: e.memset(z[:], 0.0), writes=['zz'])
            for fb in range(4):
                dma(lambda e, fb=fb: e.dma_start(out=YT[1][fb * 128:(fb + 1) * 128, :], in_=z[:]), reads=['zz'])
            kb.end()
        stage_on(6)
        chunks6 = CHUNKS[1:] if last else CHUNKS
        kb.begin()
        wbc = [kb.sb("wbc%d" % i, [128, 16, 128]) for i in range(2)]; wo = kb.sb("wo", [128, 8, D])
        yT = [kb.sb("yT%d" % i, [128, 4, 512]) for i in range(4)]
        gT = [kb.sb("gT%d" % i, [128, 512]) for i in range(2)]
        tmp = kb.sb("tmp6", [128, 512]); acc = kb.sb("acc", [128, 8, 512])
        xt = kb.sb("xt6", [128, D]); z = kb.sb("z6", [128, D]); z2 = kb.sb("z62", [128, D]); st = kb.sb("st6", [128, 4])
        pb = [kb.ps("pb%d" % i, [128, 512]) for i in range(2)]
        pmx = [kb.ps("pmx%d" % i, [128, 512]) for i in range(2)]
        dma(lambda e: e.dma_start(out=wo[:], in_=w_out[l].rearrange("(k p) n -> p k n", p=128)), writes=['wo'])

        def layer_norm(src, srck, dst, dstk, g, b, eps, n_=D):
            op('dve', lambda e: e.reduce_sum(st[:, 0:1], src[:], axis=AX.X), reads=[srck], writes=['st6'])
            op('dve', lambda e: e.tensor_scalar(st[:, 1:2], st[:, 0:1], -1.0 / n_, None, op0=ALU.mult), reads=['st6'], writes=['st6'])
            op('dve', lambda e: e.tensor_scalar(src[:], src[:], st[:, 1:2], None, op0=ALU.add), reads=['st6', srck], writes=[srck])
            op('dve', lambda e: e.scalar_tensor_tensor(out=dst[:], in0=src[:], scalar=1.0, in1=src[:], op0=ALU.mult, op1=ALU.mult, accum_out=st[:, 2:3]),
               reads=[srck], writes=[dstk, 'st6'])
            op('dve', lambda e: e.tensor_scalar(st[:, 3:4], st[:, 2:3], 1.0 / n_, eps, op0=ALU.mult, op1=ALU.add), reads=['st6'], writes=['st6'])
            op('act', lambda e: e.activation(st[:, 3:4], st[:, 3:4], AF.Sqrt), reads=['st6'], writes=['st6'])
            op('dve', lambda e: e.reciprocal(st[:, 3:4], st[:, 3:4]), reads=['st6'], writes=['st6'])
            op('dve', lambda e: e.scalar_tensor_tensor(out=dst[:], in0=src[:], scalar=st[:, 3:4], in1=g[:], op0=ALU.mult, op1=ALU.mult), reads=[srck, 'st6'], writes=[dstk])
            op('pool', lambda e: e.tensor_tensor(out=dst[:], in0=dst[:], in1=b[:], op=ALU.add), reads=[dstk], writes=[dstk])

        gc = 0
        for (n0, n) in chunks6:
            which = 1 if n0 == 0 else 0
            for i in range(4):
                dma(lambda e, i=i, n0=n0, n=n: e.dma_start(out=yT[i][:, :, 0:n], in_=YT[i][:, n0:n0 + n].rearrange("(k p) n -> p k n", p=128)), writes=['yT%d' % i])
            for c in range(8):
                WB = wbc[c % 2]; wbk = 'wbc%d' % (c % 2)
                dma(lambda e, WB=WB, c=c: e.dma_start(out=WB[:], in_=w_branch[l, :, :, c * 128:(c + 1) * 128].rearrange("i (k p) n -> p (i k) n", p=128)), writes=[wbk])
                for i in range(4):
                    G_ = gT[gc % 2]; gk_ = 'gT%d' % (gc % 2); PB = pb[gc % 2]; pbk = 'pb%d' % (gc % 2); gc += 1
                    r0 = OFF_GATE + i * D + c * 128
                    dma(lambda e, G_=G_, r0=r0, n0=n0, n=n: e.dma_start(out=G_[:, 0:n], in_=PT[r0:r0 + 128, n0:n0 + n]), writes=[gk_])
                    op('act', lambda e, G_=G_, n=n: e.activation(G_[:, 0:n], G_[:, 0:n], AF.Sigmoid), reads=[gk_], writes=[gk_])
                    for k in range(4):
                        op('pe', lambda e, PB=PB, WB=WB, i=i, k=k, n=n: e.matmul(PB[:, 0:n], WB[:, i * 4 + k, :], yT[i][:, k, 0:n], start=(k == 0), stop=(k == 3)),
                           reads=[wbk, 'yT%d' % i], writes=[pbk])
                    if i == 0:
                        op('dve', lambda e, PB=PB, G_=G_, c=c, n=n: e.tensor_tensor(out=acc[:, c, 0:n], in0=PB[:, 0:n], in1=G_[:, 0:n], op=ALU.mult), reads=[pbk, gk_], writes=['acc'])
                    else:
                        op('dve', lambda e, PB=PB, G_=G_, n=n: e.tensor_tensor(out=tmp[:, 0:n], in0=PB[:, 0:n], in1=G_[:, 0:n], op=ALU.mult), reads=[pbk, gk_], writes=['tmp6'])
                        op('pool', lambda e, c=c, n=n: e.tensor_tensor(out=acc[:, c, 0:n], in0=acc[:, c, 0:n], in1=tmp[:, 0:n], op=ALU.add), reads=['tmp6', 'acc'], writes=['acc'])
            for sub in range(n // 128):
                t = n0 // 128 + sub
                dma(lambda e, t=t: e.dma_start(out=xt[:], in_=XS[t * 128:(t + 1) * 128, :]), writes=['xt6'])
                for hf in range(2):
                    PM = pmx[hf]; pmk = 'pmx%d' % hf
                    for c in range(8):
                        op('pe', lambda e, PM=PM, c=c, sub=sub, hf=hf: e.matmul(PM[:], acc[:, c, sub * 128:(sub + 1) * 128], wo[:, c, hf * 512:(hf + 1) * 512], start=(c == 0), stop=(c == 7)),
                           reads=['acc', 'wo'], writes=[pmk])
                    op('dve', lambda e, PM=PM, hf=hf, which=which: e.tensor_tensor(out=z2[:, hf * 512:(hf + 1) * 512], in0=PM[:], in1=bc["gt1"][which][:, hf * 512:(hf + 1) * 512], op=ALU.mult),
                       reads=[pmk], writes=['z62'])
                op('dve', lambda e: e.scalar_tensor_tensor(out=z[:], in0=xt[:], scalar=ALPHA, in1=z2[:], op0=ALU.mult, op1=ALU.add), reads=['xt6', 'z62'], writes=['z6'])
                layer_norm(z, 'z6', z2, 'z62', lnp["g1"], lnp["b1"], 1e-5)
                dma(lambda e, t=t: e.dma_start(out=XMID[t * 128:(t + 1) * 128, :], in_=z2[:]), reads=['z62'])
        kb.end()
        if stop_after == 's6':
            break
        stage_on(7)
        kb.begin()
        tiles7 = list(range(2, NT)) if last else list(range(NT))
        if peer_tiles is not None:
            tiles7 = list(peer_tiles)
        wq = kb.sb("wq7", [128, 8, D]); keysT = kb.sb("keysT", [128, 8, 256]); kraw = kb.sb("kraw", [128, 16, 64])
        xm = kb.sb("xm7", [128, D]); hh = kb.sb("hh7", [128, D]); hT = kb.sb("hT7", [128, 8, 128]); qT = kb.sb("qT7", [128, 128])
        sc = kb.sb("sc7", [128, 16, 128]); wk = kb.sb("wk7", [128, 256])
        v16 = kb.sb("v16", [128, 16, 16]); i16 = kb.sb("i16", [128, 16, 16], U32); i16f = kb.sb("i16f", [128, 16, 16])
        cand = kb.sb("cand", [128, 8, 256]); best = kb.sb("best", [128, 8, 16]); pos = kb.sb("pos", [128, 8, 16], U32)
        pa = kb.sb("pa", [128, 8, 16], U32); pbi = kb.sb("pbi", [128, 8, 16], U32); paf = kb.sb("paf", [128, 8, 16]); pbf = kb.sb("pbf", [128, 8, 16])
        eq = kb.sb("eq7", [128, 8, 16, 16]); id1 = kb.sb("id1", [128, 8, 16]); id2 = kb.sb("id2", [128, 8, 16])
        eidf = kb.sb("eidf", [128, 128]); eid = kb.sb("eid", [128, 128], I32)
        wg = kb.sb("wg7", [128, 8, 16]); ssum = kb.sb("ssum7", [128, 8]); iota16 = kb.sb("iota16", [128, 16])
        apre = kb.sb("apre", [128, 128]); coef = kb.sb("coef", [128, 128])
        NB = 6
        rows = [kb.sb("row%d" % i, [128, D]) for i in range(NB)]
        junk = kb.sb("junk7", [128, D]); facc = kb.sb("facc", [128, D]); st = kb.sb("st6", [128, 4]); z = kb.sb("z7", [128, D])
        ptx = [kb.ps("p7t%d" % i, [128, 128]) for i in range(2)]
        pq7 = kb.ps("pq7", [128, 128]); psc = kb.ps("psc7", [128, 16, 128])
        dma(lambda e: e.dma_start(out=wq[:], in_=peer_wq[l].rearrange("(k p) n -> p k n", p=128)), writes=['wq7'])
        dma(lambda e: e.dma_start(out=kraw[:], in_=peer_keys[l].rearrange("h s k d -> k (h s) d")), writes=['kraw'])
        dma(lambda e: e.dma_start(out=iota16[:], in_=c_iota16), writes=['iota16'])
        op('pool', lambda e: e.memset(keysT[:], 0.0), writes=['keysT'])
        for h in range(8):
            op('pe', lambda e, h=h: e.transpose(ptx[h % 2][:], kraw[:, 2 * h:2 * h + 2, :].rearrange("p a d -> p (a d)"), ident[:]), reads=['kraw'], writes=['p7t%d' % (h % 2)])
            for s_ in range(2):
                op('act', lambda e, h=h, s_=s_: e.copy(keysT[64 * s_:64 * s_ + 64, h, 128 * s_:128 * s_ + 128], ptx[h % 2][64 * s_:64 * s_ + 64, :]), reads=['p7t%d' % (h % 2)], writes=['keysT'])
        rc = 0
        for t in tiles7:
            sub7(2)
            which = 1 if t < 2 else 0
            dma(lambda e, t=t: e.dma_start(out=xm[:], in_=XMID[t * 128:(t + 1) * 128, :]), writes=['xm7'])
            op('dve', lambda e, which=which: e.tensor_tensor(out=hh[:], in0=xm[:], in1=bc["sc2"][which][:], op=ALU.mult), reads=['xm7'], writes=['hh7'])
            op('dve', lambda e, which=which: e.tensor_tensor(out=hh[:], in0=hh[:], in1=bc["sh2"][which][:], op=ALU.add), reads=['hh7'], writes=['hh7'])
            for k in range(8):
                op('pe', lambda e, k=k: e.transpose(ptx[k % 2][:], hh[:, k * 128:(k + 1) * 128], ident[:]), reads=['hh7'], writes=['p7t%d' % (k % 2)])
                op('act', lambda e, k=k: e.copy(hT[:, k, :], ptx[k % 2][:]), reads=['p7t%d' % (k % 2)], writes=['hT7'])
            for h in range(8):
                sub7(2.3)
                for k in range(8):
                    op('pe', lambda e, h=h, k=k: e.matmul(pq7[:], wq[:, k, h * 128:(h + 1) * 128], hT[:, k, :], start=(k == 0), stop=(k == 7)), reads=['wq7', 'hT7'], writes=['pq7'])
                op('act', lambda e: e.copy(qT[:], pq7[:]), reads=['pq7'], writes=['qT7'])
                sub7(2.6)
                op('pe', lambda e, h=h: e.matmul(psc[:, 2 * h:2 * h + 2, :].rearrange("p a k -> p (a k)"), qT[:, :], keysT[:, h, :], start=True, stop=True),
                   reads=['qT7', 'keysT'], writes=['psc7'])
            sub7(3)
            for q4 in range(4):
                op('act', lambda e, q4=q4: e.copy(sc[:, 4 * q4:4 * q4 + 4, :], psc[:, 4 * q4:4 * q4 + 4, :]), reads=['psc7'], writes=['sc7'])

            def top16(src, vdst, idst):
                N = src.shape[-1]
                op('dve', lambda e: e.max(out=vdst[:, 0:8], in_=src), reads=['sc7', 'cand'], writes=['v16'])
                op('dve', lambda e: e.match_replace(out=wk[:, 0:N], in_to_replace=vdst[:, 0:8], in_values=src, imm_value=-1e30), reads=['v16', 'sc7', 'cand'], writes=['wk7'])
                op('dve', lambda e: e.max(out=vdst[:, 8:16], in_=wk[:, 0:N]), reads=['wk7'], writes=['v16'])
                op('dve', lambda e: e.max_index(out=idst[:, 0:8], in_max=vdst[:, 0:8], in_values=src), reads=['v16', 'sc7', 'cand'], writes=['i16'])
                op('dve', lambda e: e.max_index(out=idst[:, 8:16], in_max=vdst[:, 8:16], in_values=src), reads=['v16', 'sc7', 'cand'], writes=['i16'])

            sub7(4)
            for j in range(16):
                top16(sc[:, j, :], v16[:, j, :], i16[:, j, :])
            v4 = v16[:].rearrange("p (h s) a -> p h s a", s=2)
            op('dve', lambda e: e.tensor_tensor(out=cand[:].rearrange("p h (a b) -> p h a b", b=16), in0=v4[:, :, 0, :].unsqueeze(3).to_broadcast([128, 8, 16, 16]),
                                                 in1=v4[:, :, 1, :].unsqueeze(2).to_broadcast([128, 8, 16, 16]), op=ALU.add), reads=['v16'], writes=['cand'])
            for h in range(8):
                top16(cand[:, h, :], best[:, h, :], pos[:, h, :])
            sub7(5)
            op('dve', lambda e: e.tensor_tensor(out=wg[:], in0=best[:], in1=best[:, :, 0:1].to_broadcast([128, 8, 16]), op=ALU.subtract), reads=['v16'], writes=['wg7'])
            op('act', lambda e: e.activation(wg[:], wg[:], AF.Exp), reads=['wg7'], writes=['wg7'])
            op('dve', lambda e: e.reduce_sum(ssum[:], wg[:], axis=AX.X), reads=['wg7'], writes=['ssum7'])
            op('dve', lambda e: e.reciprocal(ssum[:], ssum[:]), reads=['ssum7'], writes=['ssum7'])
            op('dve', lambda e: e.tensor_tensor(out=wg[:], in0=wg[:], in1=ssum[:].unsqueeze(2).to_broadcast([128, 8, 16]), op=ALU.mult), reads=['wg7', 'ssum7'], writes=['wg7'])
            op('dve', lambda e: e.tensor_single_scalar(pa[:], pos[:], 4, op=ALU.logical_shift_right), reads=['i16'], writes=['pa'])
            op('dve', lambda e: e.tensor_single_scalar(pbi[:], pos[:], 15, op=ALU.bitwise_and), reads=['i16'], writes=['pbi'])
            op('dve', lambda e: e.tensor_copy(paf[:], pa[:]), reads=['pa'], writes=['paf'])
            op('dve', lambda e: e.tensor_copy(pbf[:], pbi[:]), reads=['pbi'], writes=['pbf'])
            op('dve', lambda e: e.tensor_copy(i16f[:], i16[:]), reads=['i16'], writes=['i16f'])
            i4 = i16f[:].rearrange("p (h s) a -> p h s a", s=2)
            for (pf, s, dst, dk) in ((paf, 0, id1, 'id1'), (pbf, 1, id2, 'id2')):
                op('dve', lambda e, pf=pf: e.tensor_tensor(out=eq[:], in0=pf[:].unsqueeze(3).to_broadcast([128, 8, 16, 16]),
                                                          in1=iota16[:].unsqueeze(1).unsqueeze(1).to_broadcast([128, 8, 16, 16]), op=ALU.is_equal),
                   reads=['paf', 'pbf', 'iota16'], writes=['eq7'])
                op('dve', lambda e, s=s: e.tensor_tensor(out=eq[:], in0=eq[:], in1=i4[:, :, s, :].unsqueeze(2).to_broadcast([128, 8, 16, 16]), op=ALU.mult),
                   reads=['eq7', 'i16f'], writes=['eq7'])
                op('dve', lambda e, dst=dst: e.reduce_sum(dst[:], eq[:], axis=AX.X), reads=['eq7'], writes=[dk])
            op('dve', lambda e: e.scalar_tensor_tensor(out=eidf[:], in0=id1[:].rearrange("p h k -> p (h k)"), scalar=128.0, in1=id2[:].rearrange("p h k -> p (h k)"), op0=ALU.mult, op1=ALU.add),
               reads=['id1', 'id2'], writes=['eidf'])
            if l > 0:
                op('dve', lambda e: e.tensor_scalar_add(eidf[:], eidf[:], float(l * 16384)), reads=['eidf'], writes=['eidf'])
            op('dve', lambda e: e.tensor_scalar(eidf[:], eidf[:], 0.0, 32767.0, op0=ALU.max, op1=ALU.min), reads=['eidf'], writes=['eidf'])
            op('dve', lambda e: e.tensor_copy(eid[:], eidf[:]), reads=['eidf'], writes=['eid'])
            sub7(6)
            for e_ in range(peer_slots):
                R = rows[rc % NB]; rk_ = 'row%d' % (rc % NB); rc += 1
                dma(lambda e, R=R, e_=e_: e.indirect_dma_start(out=R[:], out_offset=None, in_=peer_u.rearrange("l e d -> (l e) d"), in_offset=bass.IndirectOffsetOnAxis(ap=eid[:, e_:e_ + 1], axis=0)),
                    reads=['eid'], writes=[rk_], q='pool')
                op('dve', lambda e, R=R, e_=e_: e.scalar_tensor_tensor(out=junk[:], in0=R[:], scalar=1.0, in1=hh[:], op0=ALU.mult, op1=ALU.mult, accum_out=apre[:, e_:e_ + 1]),
                   reads=[rk_, 'hh7'], writes=['junk7', 'apre'])
            sub7(7)
            op('act', lambda e: e.activation(coef[:], apre[:], AF.Gelu), reads=['apre'], writes=['coef'])
            op('dve', lambda e: e.tensor_tensor(out=coef[:], in0=coef[:], in1=wg[:].rearrange("p h k -> p (h k)"), op=ALU.mult), reads=['coef', 'wg7'], writes=['coef'])
            for e_ in range(peer_slots):
                R = rows[rc % NB]; rk_ = 'row%d' % (rc % NB); rc += 1
                dma(lambda e, R=R, e_=e_: e.indirect_dma_start(out=R[:], out_offset=None, in_=peer_v.rearrange("l e d -> (l e) d"), in_offset=bass.IndirectOffsetOnAxis(ap=eid[:, e_:e_ + 1], axis=0)),
                    reads=['eid'], writes=[rk_], q='pool')
                if e_ == 0:
                    op('dve', lambda e, R=R: e.tensor_scalar(facc[:], R[:], coef[:, 0:1], None, op0=ALU.mult), reads=[rk_, 'coef'], writes=['facc'])
                else:
                    op('dve', lambda e, R=R, e_=e_: e.scalar_tensor_tensor(out=facc[:], in0=R[:], scalar=coef[:, e_:e_ + 1], in1=facc[:], op0=ALU.mult, op1=ALU.add),
                       reads=[rk_, 'coef', 'facc'], writes=['facc'])
            op('dve', lambda e, which=which: e.tensor_tensor(out=facc[:], in0=facc[:], in1=bc["gt2"][which][:], op=ALU.mult), reads=['facc'], writes=['facc'])
            op('dve', lambda e: e.scalar_tensor_tensor(out=z[:], in0=xm[:], scalar=ALPHA, in1=facc[:], op0=ALU.mult, op1=ALU.add), reads=['xm7', 'facc'], writes=['z7'])
            layer_norm(z, 'z7', facc, 'facc', lnp["g2"], lnp["b2"], 1e-5)
            if last:
                dma(lambda e, t=t: e.dma_start(out=yout[(t - 2) * 128:(t - 1) * 128, :], in_=facc[:]), reads=['facc'])
            else:
                dma(lambda e, t=t: e.dma_start(out=XN[t * 128:(t + 1) * 128, :], in_=facc[:]), reads=['facc'])
        stage_on(7)
        kb.end()
        kb.end()
    p.barrier()
    return kb


def rwkv_stage(kb, l, PT, Yd, rw, cst, last, scan_steps=None, parts=None):
    nc, p = kb.nc, kb.p
    on = [True]

    def op(*a, **k):
        if on[0]:
            p.op(*a, **k)

    def dma(*a, **k):
        if on[0]:
            p.dma(*a, **k)

    def part(x):
        on[0] = parts is None or x in parts
    ident, ones, bones, c_hmask = cst['ident'], cst['ones'], cst['bones'], cst['c_hmask']
    sc = kb.scratch
    RM = sc("RM%d" % l, [1920, TOK])
    KKT = sc("KKT%d" % l, [512, TOK]); WT = [sc("WT%d_%d" % (l, d), [512, TOK]) for d in range(2)]
    TR = sc("TR%d" % l, [TOK, 512]); TV = sc("TV%d" % l, [TOK, 512]); TG = sc("TG%d" % l, [TOK, 512])
    TKD = [sc("TKD%d_%d" % (l, d), [TOK, 512]) for d in range(2)]
    TKA = [sc("TKA%d_%d" % (l, d), [TOK, 512]) for d in range(2)]
    YS = [sc("YS%d_%d" % (l, d), [8, TOK, 64]) for d in range(2)]
    part('A')
    kb.begin()
    XP = [kb.sb("XP%d" % i, [128, TOK + 8]) for i in range(2)]
    MO = [kb.sb("MO%d" % i, [128, TOK]) for i in range(2)]
    mu = kb.sb("mu", [128, 15, 2]); c0 = kb.sb("c0", [128, 15])
    for j in range(2):
        dma(lambda e, j=j: e.dma_start(out=mu[:, :, j], in_=rw["rwkv_mu"][l, j].rearrange("(c p) -> p c", p=128), allow_slow_non_contiguous=True), writes=['mu'])
    op('dve', lambda e: e.tensor_tensor(out=c0[:], in0=mu[:, :, 0], in1=mu[:, :, 1], op=ALU.add), reads=['mu'], writes=['c0'])
    op('dve', lambda e: e.tensor_scalar(c0[:], c0[:], -1.0, 1.0, op0=ALU.mult, op1=ALU.add), reads=['c0'], writes=['c0'])
    for i in range(2):
        op('pool', lambda e, i=i: e.memset(XP[i][:], 0.0), writes=['XP%d' % i])
    for c in range(15):
        X = XP[c % 2]; xk = 'XP%d' % (c % 2); M = MO[c % 2]; mk = 'MO%d' % (c % 2)
        r0 = OFF_RWKV + c * 128
        dma(lambda e, X=X, r0=r0: e.dma_start(out=X[:, 1:257], in_=PT[r0:r0 + 128, 0:256]), writes=[xk])
        dma(lambda e, X=X, r0=r0: e.dma_start(out=X[:, 259:259 + SEQ], in_=PT[r0:r0 + 128, 256:TOK]), writes=[xk])
        for (o0, n, x0) in ((0, 256, 1), (256, SEQ, 259)):
            op('dve', lambda e, X=X, M=M, o0=o0, n=n, x0=x0, c=c: e.tensor_scalar(M[:, o0:o0 + n], X[:, x0:x0 + n], c0[:, c:c + 1], None, op0=ALU.mult), reads=[xk, 'c0'], writes=[mk])
            op('dve', lambda e, X=X, M=M, o0=o0, n=n, x0=x0, c=c: e.scalar_tensor_tensor(out=M[:, o0:o0 + n], in0=X[:, x0 - 1:x0 - 1 + n], scalar=mu[:, c, 0:1], in1=M[:, o0:o0 + n], op0=ALU.mult, op1=ALU.add),
               reads=[xk, 'mu', mk], writes=[mk])
            op('dve', lambda e, X=X, M=M, o0=o0, n=n, x0=x0, c=c: e.scalar_tensor_tensor(out=M[:, o0:o0 + n], in0=X[:, x0 + 1:x0 + 1 + n], scalar=mu[:, c, 1:2], in1=M[:, o0:o0 + n], op0=ALU.mult, op1=ALU.add),
               reads=[xk, 'mu', mk], writes=[mk])
        dma(lambda e, M=M, c=c: e.dma_start(out=RM[c * 128:(c + 1) * 128, :], in_=M[:]), reads=[mk])
    kb.end()
    part('B')
    kb.begin()
    pv_ = {}
    def col(name, src, ncol):
        t = kb.sb(name, [128, ncol])
        dma(lambda e: e.dma_start(out=t[:], in_=src.rearrange("(c p) -> p c", p=128), allow_slow_non_contiguous=True), writes=[name])
        return t
    kkc = col("kkc", rw["rwkv_k_k"][l], 4); kac = col("kac", rw["rwkv_k_a"][l], 4); rkc = col("rkc", rw["rwkv_r_k"][l], 4)
    w0c = [col("w0c%d" % d, rw["rwkv_w0"][l, d], 4) for d in range(2)]
    a0c = [col("a0c%d" % d, rw["rwkv_a0"][l, d], 4) for d in range(2)]
    omka = kb.sb("omka", [128, 4])
    op('dve', lambda e: e.tensor_scalar(omka[:], kac[:], -1.0, 1.0, op0=ALU.mult, op1=ALU.add), reads=['kac'], writes=['omka'])
    w2 = kb.sb("w2", [128, 512]); a2 = kb.sb("a2", [128, 512]); g2 = kb.sb("g2", [128, 512])
    dma(lambda e: e.dma_start(out=w2[:], in_=rw["rwkv_w2"][l].rearrange("d k n -> (d k) n")), writes=['w2'])
    dma(lambda e: e.dma_start(out=a2[:], in_=rw["rwkv_a2"][l].rearrange("d k n -> (d k) n")), writes=['a2'])
    dma(lambda e: e.dma_start(out=g2[:], in_=rw["rwkv_g2"][l]), writes=['g2'])
    Rt = kb.sb("Rt", [128, 4, 512]); Kt = kb.sb("Kt", [128, 4, 512]); Vt = kb.sb("Vt", [128, 4, 512])
    wfb = kb.sb("wfb", [128, 512]); afb = kb.sb("afb", [128, 512]); gi = kb.sb("gi", [128, 512])
    kk = kb.sb("kkB", [128, 4, 512]); tB = kb.sb("tB", [128, 512]); sqB = kb.sb("sqB", [128, 512]); nB = kb.sb("nB", [128, 512])
    aT = kb.sb("aT", [128, 512]); fo = [kb.sb("fo%d" % i, [128, 512]) for i in range(3)]
    tm = {nm: kb.sb("tm_" + nm, [128, 4, 512]) for nm in ("kd0", "kd1", "ka0", "ka1", "g")}
    tok = [kb.sb("tokB%d" % i, [128, 512]) for i in range(2)]
    pB = [kb.ps("pB%d" % i, [128, 512]) for i in range(3)]
    pT = [kb.ps("pTB%d" % i, [128, 512]) for i in range(2)]
    fc = 0; tc_ = 0
    NEG = -math.exp(-0.5)
    for (n0, n) in CHUNKS:
        for (T, nm, r0) in ((Rt, 'Rt', 0), (Kt, 'Kt', 512), (Vt, 'Vt', 1024)):
            dma(lambda e, T=T, r0=r0, n0=n0, n=n: e.dma_start(out=T[:, :, 0:n], in_=RM[r0:r0 + 512, n0:n0 + n].rearrange("(c p) n -> p c n", p=128)), writes=[nm])
        dma(lambda e, n0=n0, n=n: e.dma_start(out=wfb[:, 0:n], in_=RM[1536:1664, n0:n0 + n]), writes=['wfb'])
        dma(lambda e, n0=n0, n=n: e.dma_start(out=afb[:, 0:n], in_=RM[1664:1792, n0:n0 + n]), writes=['afb'])
        dma(lambda e, n0=n0, n=n: e.dma_start(out=gi[:, 0:n], in_=RM[1792:1920, n0:n0 + n]), writes=['gi'])
        op('act', lambda e, n=n: e.activation(wfb[:, 0:n], wfb[:, 0:n], AF.Tanh), reads=['wfb'], writes=['wfb'])
        op('act', lambda e, n=n: e.activation(gi[:, 0:n], gi[:, 0:n], AF.Sigmoid), reads=['gi'], writes=['gi'])
        for c in range(4):
            op('dve', lambda e, c=c, n=n: e.tensor_scalar(tB[:, 0:n], Kt[:, c, 0:n], kkc[:, c:c + 1], None, op0=ALU.mult), reads=['Kt', 'kkc'], writes=['tB'])
            op('act', lambda e, n=n: e.activation(sqB[:, 0:n], tB[:, 0:n], AF.Square), reads=['tB'], writes=['sqB'])
            P_ = pB[fc % 3]; pk_ = 'pB%d' % (fc % 3); fc += 1
            op('pe', lambda e, P_=P_, n=n: e.matmul(P_[:, 0:n], bones[:], sqB[:, 0:n], start=True, stop=True), reads=['sqB'], writes=[pk_])
            op('act', lambda e, P_=P_, n=n: e.activation(nB[:, 0:n], P_[:, 0:n], AF.Sqrt), reads=[pk_], writes=['nB'])
            op('dve', lambda e, n=n: e.tensor_scalar(nB[:, 0:n], nB[:, 0:n], 1e-12, None, op0=ALU.max), reads=['nB'], writes=['nB'])
            op('dve', lambda e, n=n: e.reciprocal(nB[:, 0:n], nB[:, 0:n]), reads=['nB'], writes=['nB'])
            op('dve', lambda e, c=c, n=n: e.tensor_tensor(out=kk[:, c, 0:n], in0=tB[:, 0:n], in1=nB[:, 0:n], op=ALU.mult), reads=['tB', 'nB'], writes=['kkB'])
            for d in range(2):
                P_ = pB[fc % 3]; pk_ = 'pB%d' % (fc % 3); fc += 1
                F = fo[fc % 3]; fk = 'fo%d' % (fc % 3)
                op('pe', lambda e, P_=P_, c=c, d=d, n=n: e.matmul(P_[:, 0:n], w2[64 * d:64 * d + 64, c * 128:(c + 1) * 128], wfb[64 * d:64 * d + 64, 0:n], start=True, stop=True),
                   reads=['w2', 'wfb'], writes=[pk_])
                op('act', lambda e, P_=P_, F=F, c=c, d=d, n=n: e.activation(F[:, 0:n], P_[:, 0:n], AF.Sigmoid, bias=w0c[d][:, c:c + 1]), reads=[pk_], writes=[fk])
                op('act', lambda e, F=F, n=n: e.activation(F[:, 0:n], F[:, 0:n], AF.Exp, scale=NEG), reads=[fk], writes=[fk])
                dma(lambda e, F=F, c=c, d=d, n0=n0, n=n: e.dma_start(out=WT[d][c * 128:(c + 1) * 128, n0:n0 + n], in_=F[:, 0:n]), reads=[fk])
                P_ = pB[fc % 3]; pk_ = 'pB%d' % (fc % 3); fc += 1
                op('pe', lambda e, P_=P_, c=c, d=d, n=n: e.matmul(P_[:, 0:n], a2[64 * d:64 * d + 64, c * 128:(c + 1) * 128], afb[64 * d:64 * d + 64, 0:n], start=True, stop=True),
                   reads=['a2', 'afb'], writes=[pk_])
                op('act', lambda e, P_=P_, c=c, d=d, n=n: e.activation(aT[:, 0:n], P_[:, 0:n], AF.Sigmoid, bias=a0c[d][:, c:c + 1]), reads=[pk_], writes=['aT'])
                op('dve', lambda e, c=c, d=d, n=n: e.scalar_tensor_tensor(out=tm["ka%d" % d][:, c, 0:n], in0=kk[:, c, 0:n], scalar=-1.0, in1=aT[:, 0:n], op0=ALU.mult, op1=ALU.mult),
                   reads=['kkB', 'aT'], writes=['tm_ka%d' % d])
                op('dve', lambda e, c=c, n=n: e.tensor_scalar(aT[:, 0:n], aT[:, 0:n], kac[:, c:c + 1], omka[:, c:c + 1], op0=ALU.mult, op1=ALU.add), reads=['aT', 'kac', 'omka'], writes=['aT'])
                op('dve', lambda e, c=c, d=d, n=n: e.tensor_tensor(out=tm["kd%d" % d][:, c, 0:n], in0=Kt[:, c, 0:n], in1=aT[:, 0:n], op=ALU.mult), reads=['Kt', 'aT'], writes=['tm_kd%d' % d])
            dma(lambda e, c=c, n0=n0, n=n: e.dma_start(out=KKT[c * 128:(c + 1) * 128, n0:n0 + n], in_=kk[:, c, 0:n]), reads=['kkB'])
            P_ = pB[fc % 3]; pk_ = 'pB%d' % (fc % 3); fc += 1
            op('pe', lambda e, P_=P_, c=c, n=n: e.matmul(P_[:, 0:n], g2[:, c * 128:(c + 1) * 128], gi[:, 0:n], start=True, stop=True), reads=['g2', 'gi'], writes=[pk_])
            op('act', lambda e, P_=P_, c=c, n=n: e.copy(tm["g"][:, c, 0:n], P_[:, 0:n]), reads=[pk_], writes=['tm_g'])
        for (src, sk, dst) in ((Rt, 'Rt', TR), (Vt, 'Vt', TV), (tm["g"], 'tm_g', TG), (tm["kd0"], 'tm_kd0', TKD[0]), (tm["kd1"], 'tm_kd1', TKD[1]),
                               (tm["ka0"], 'tm_ka0', TKA[0]), (tm["ka1"], 'tm_ka1', TKA[1])):
            for sub in range(n // 128):
                P_ = pT[tc_ % 2]; pk_ = 'pTB%d' % (tc_ % 2); O = tok[tc_ % 2]; ok_ = 'tokB%d' % (tc_ % 2); tc_ += 1
                for c in range(4):
                    op('pe', lambda e, P_=P_, src=src, c=c, sub=sub: e.transpose(P_[:, c * 128:(c + 1) * 128], src[:, c, sub * 128:(sub + 1) * 128], ident[:]), reads=[sk], writes=[pk_])
                if tc_ % 2:
                    op('act', lambda e, P_=P_, O=O: e.copy(O[:], P_[:]), reads=[pk_], writes=[ok_])
                else:
                    op('dve', lambda e, P_=P_, O=O: e.tensor_copy(O[:], P_[:]), reads=[pk_], writes=[ok_])
                t0 = n0 + sub * 128
                dma(lambda e, O=O, dst=dst, t0=t0: e.dma_start(out=dst[t0:t0 + 128, :], in_=O[:]), reads=[ok_])
    kb.end()
    part('C')
    kb.begin()
    TS = 8
    S = kb.sb("S", [128, 512]); tS = kb.sb("tS", [128, 512]); hmask = kb.sb("hmask", [128, 512]); r2a = kb.sb("r2a", [128, 512])
    A = [kb.sb("A%d" % i, [128, TS, 40]) for i in range(2)]
    sKK = [kb.sb("sKK%d" % i, [128, 8, TS + 1]) for i in range(2)]
    sR = [kb.sb("sR%d" % i, [128, 8, TS]) for i in range(2)]
    sW = [kb.sb("sW%d" % i, [128, 8, TS]) for i in range(2)]
    Ua = [kb.sb("Ua%d" % i, [128, TS, 64]) for i in range(2)]
    Uk = [kb.sb("Uk%d" % i, [128, TS, 64]) for i in range(2)]
    V2 = [kb.sb("V2%d" % i, [128, TS, 512]) for i in range(2)]
    Yb = [kb.sb("Yb%d" % i, [128, TS, 512]) for i in range(2)]
    ya = [kb.sb("ya%d" % i, [128, TS, 64]) for i in range(2)]
    P1 = [kb.ps("P1_%d" % i, [128, 512]) for i in range(2)]
    PU = [kb.ps("PU_%d" % i, [128, 512]) for i in range(2)]
    dma(lambda e: e.dma_start(out=hmask[:], in_=c_hmask), writes=['hmask'])
    op('pool', lambda e: e.memset(S[:], 0.0), writes=['S'])
    op('pool', lambda e: e.memset(r2a[:], 0.0), writes=['r2a'])
    for i in range(2):
        for (T, nm) in ((A, 'A'), (Ua, 'Ua'), (Uk, 'Uk'), (V2, 'V2'), (sKK, 'sKK')):
            op('pool', lambda e, T=T, i=i: e.memset(T[i][:], 0.0), writes=['%s%d' % (nm, i)])
    nsteps = scan_steps or TOK
    def tok_of(d, s):
        if d == 0:
            return s
        return 255 - s if s < 256 else 4607 - s
    for ch in range(nsteps // TS):
        b = ch % 2
        s0 = ch * TS
        lo = [min(tok_of(d, s0), tok_of(d, s0 + TS - 1)) for d in range(2)]
        for d in range(2):
            pr = slice(64 * d, 64 * d + 64)
            rr = slice(64 * d, 64 * d + 8)
            l0 = lo[d]
            if d == 0:
                nload = TS + 1 if l0 + TS < TOK else TS
                dma(lambda e, pr=pr, l0=l0, nload=nload, b=b: e.dma_start(out=sKK[b][pr, :, 0:nload], in_=KKT[:, l0:l0 + nload].rearrange("(h j) t -> j h t", j=64)), writes=['sKK%d' % b])
            else:
                if l0 >= 1 and l0 != 256:
                    dma(lambda e, pr=pr, l0=l0, b=b: e.dma_start(out=sKK[b][pr, :, 0:TS + 1], in_=KKT[:, l0 - 1:l0 + TS].rearrange("(h j) t -> j h t", j=64)), writes=['sKK%d' % b])
                else:
                    dma(lambda e, pr=pr, l0=l0, b=b: e.dma_start(out=sKK[b][pr, :, 1:TS + 1], in_=KKT[:, l0:l0 + TS].rearrange("(h j) t -> j h t", j=64)), writes=['sKK%d' % b])
                    if l0 == 0:
                        dma(lambda e, pr=pr, b=b: e.dma_start(out=sKK[b][pr, :, 0:1], in_=KKT[:, TOK - 1:TOK].rearrange("(h j) t -> j h t", j=64), allow_slow_non_contiguous=True), writes=['sKK%d' % b])
            dma(lambda e, pr=pr, l0=l0, b=b: e.dma_start(out=sR[b][pr, :, :], in_=RM[0:512, l0:l0 + TS].rearrange("(h j) t -> j h t", j=64)), writes=['sR%d' % b])
            dma(lambda e, pr=pr, l0=l0, b=b, d=d: e.dma_start(out=sW[b][pr, :, :], in_=WT[d][:, l0:l0 + TS].rearrange("(h j) t -> j h t", j=64)), writes=['sW%d' % b], q='pool')
            sh = 1 if d == 0 else 0
            op('act', lambda e, pr=pr, b=b, sh=sh: e.copy(A[b][pr, :, 0:8].rearrange("p t h -> p h t"), sKK[b][pr, :, sh:sh + TS]), reads=['sKK%d' % b], writes=['A%d' % b])
            op('act', lambda e, pr=pr, b=b: e.copy(A[b][pr, :, 32:40].rearrange("p t h -> p h t"), sR[b][pr, :, :]), reads=['sR%d' % b], writes=['A%d' % b])
            dma(lambda e, rr=rr, l0=l0, b=b, d=d: e.dma_start(out=Ua[b][rr, :, :], in_=TKA[d][l0:l0 + TS, :].rearrange("t (h j) -> h t j", j=64)), writes=['Ua%d' % b])
            dma(lambda e, rr=rr, l0=l0, b=b, d=d: e.dma_start(out=Uk[b][rr, :, :], in_=TKD[d][l0:l0 + TS, :].rearrange("t (h j) -> h t j", j=64)), writes=['Uk%d' % b], q='pool')
            dma(lambda e, rr=rr, l0=l0, b=b: e.dma_start(out=V2[b][rr, :, :], in_=TV[l0:l0 + TS, :].partition_broadcast(8)), writes=['V2%d' % b])
        op('pool', lambda e, b=b: e.tensor_tensor(out=V2[b][0:72, :, :], in0=V2[b][0:72, :, :], in1=hmask[0:72, :].unsqueeze(1).to_broadcast([72, TS, 512]), op=ALU.mult),
           reads=['V2%d' % b, 'hmask'], writes=['V2%d' % b])
        for si in range(TS):
            s = s0 + si
            pos = [tok_of(d, s) - lo[d] for d in range(2)]
            q = s % 2
            for d in range(2):
                pr = slice(64 * d, 64 * d + 64)
                rr = slice(64 * d, 64 * d + 8)
                op('pe', lambda e, q=q, b=b, rr=rr, pr=pr, pd=pos[d]: e.matmul(PU[q][pr, :], Uk[b][rr, pd, :], V2[b][rr, pd, :], start=True, stop=False),
                   reads=['Uk%d' % b, 'V2%d' % b], writes=['PU_%d' % q])
                op('pe', lambda e, q=q, b=b, rr=rr, pr=pr, pd=pos[d]: e.matmul(PU[q][pr, :], Ua[b][rr, pd, :], r2a[rr, :], start=False, stop=True),
                   reads=['Ua%d' % b, 'r2a'], writes=['PU_%d' % q])
            for d in range(2):
                pr = slice(64 * d, 64 * d + 64)
                op('pool', lambda e, pr=pr, b=b, pd=pos[d]: e.tensor_tensor(out=tS[pr, :].rearrange("p (h i) -> p h i", i=64), in0=S[pr, :].rearrange("p (h i) -> p h i", i=64),
                                                                          in1=sW[b][pr, :, pd].unsqueeze(2).to_broadcast([64, 8, 64]), op=ALU.mult),
                   reads=['S', 'sW%d' % b], writes=['tS'])
            op('dve', lambda e, q=q: e.tensor_tensor(out=S[:], in0=tS[:], in1=PU[q][:], op=ALU.add), reads=['tS', 'PU_%d' % q], writes=['S'])
            for d in range(2):
                pr = slice(64 * d, 64 * d + 64)
                op('pe', lambda e, q=q, b=b, pr=pr, pd=pos[d], d=d: e.matmul(P1[q][64 * d:64 * d + 40, :], A[b][pr, pd, :], S[pr, :], start=True, stop=True),
                   reads=['A%d' % b, 'S'], writes=['P1_%d' % q])
            for d in range(2):
                rr = slice(64 * d, 64 * d + 8)
                op('dve', lambda e, q=q, rr=rr: e.tensor_tensor(out=r2a[rr, :], in0=P1[q][rr, :], in1=hmask[rr, :], op=ALU.mult), reads=['P1_%d' % q, 'hmask'], writes=['r2a'])
            for d in range(2):
                yr = slice(64 * d + 32, 64 * d + 40)
                op('act', lambda e, q=q, b=b, yr=yr, pd=pos[d]: e.copy(Yb[b][yr, pd, :], P1[q][yr, :]), reads=['P1_%d' % q], writes=['Yb%d' % b])
        for d in range(2):
            yr = slice(64 * d + 32, 64 * d + 40)
            op('pool', lambda e, b=b, yr=yr: e.tensor_tensor(out=Yb[b][yr, :, :], in0=Yb[b][yr, :, :], in1=hmask[yr, :].unsqueeze(1).to_broadcast([8, TS, 512]), op=ALU.mult),
               reads=['Yb%d' % b, 'hmask'], writes=['Yb%d' % b])
            op('pool', lambda e, b=b, yr=yr: e.tensor_tensor(out=Yb[b][yr, :, 0:256], in0=Yb[b][yr, :, 0:256], in1=Yb[b][yr, :, 256:512], op=ALU.add), reads=['Yb%d' % b], writes=['Yb%d' % b])
            op('pool', lambda e, b=b, yr=yr: e.tensor_tensor(out=Yb[b][yr, :, 0:128], in0=Yb[b][yr, :, 0:128], in1=Yb[b][yr, :, 128:256], op=ALU.add), reads=['Yb%d' % b], writes=['Yb%d' % b])
            op('pool', lambda e, b=b, yr=yr: e.tensor_tensor(out=ya[b][yr, :, :], in0=Yb[b][yr, :, 0:64], in1=Yb[b][yr, :, 64:128], op=ALU.add), reads=['Yb%d' % b], writes=['ya%d' % b])
            dma(lambda e, b=b, yr=yr, d=d, l0=lo[d]: e.dma_start(out=YS[d][:, l0:l0 + TS, :], in_=ya[b][yr, :, :]), reads=['ya%d' % b])
    kb.end()
    part('D')
    kb.begin()
    lng = kb.sb("lng", [128, 512]); lnb = kb.sb("lnb", [128, 512]); rkb = kb.sb("rkb", [128, 512])
    dma(lambda e: e.dma_start(out=lng[:], in_=rw["rwkv_ln_g"][l].partition_broadcast(128)), writes=['lng'])
    dma(lambda e: e.dma_start(out=lnb[:], in_=rw["rwkv_ln_b"][l].partition_broadcast(128)), writes=['lnb'])
    dma(lambda e: e.dma_start(out=rkb[:], in_=rw["rwkv_r_k"][l].partition_broadcast(128)), writes=['rkb'])
    o0 = kb.sb("o0", [128, 8, 64]); o1 = kb.sb("o1", [128, 8, 64]); sq = kb.sb("osq", [128, 8, 64])
    mean = kb.sb("omean", [128, 8]); var = kb.sb("ovar", [128, 8])
    tr = kb.sb("otr", [128, 512]); tv = kb.sb("otv", [128, 512]); tg = kb.sb("otg", [128, 512]); k0 = kb.sb("ok0", [128, 512]); k1 = kb.sb("ok1", [128, 512])
    bon = kb.sb("obon", [128, 8])
    oT = [kb.sb("oT%d" % i, [128, 128]) for i in range(2)]
    pT = [kb.ps("pTD%d" % i, [128, 128]) for i in range(2)]
    tiles = list(range(2, NT)) if last else list(range(NT))
    if scan_steps:
        tiles = [0, 1]
    ct = 0
    v3 = lambda t: t[:].rearrange("p (h i) -> p h i", i=64)
    for t in tiles:
        t0 = t * 128
        dma(lambda e, t0=t0: e.dma_start(out=o0[:], in_=YS[0][:, t0:t0 + 128, :].rearrange("h t i -> t h i")), writes=['o0'])
        dma(lambda e, t0=t0: e.dma_start(out=o1[:], in_=YS[1][:, t0:t0 + 128, :].rearrange("h t i -> t h i")), writes=['o1'])
        for (T, nm, src) in ((tr, 'otr', TR), (tv, 'otv', TV), (tg, 'otg', TG), (k0, 'ok0', TKD[0]), (k1, 'ok1', TKD[1])):
            dma(lambda e, T=T, src=src, t0=t0: e.dma_start(out=T[:], in_=src[t0:t0 + 128, :]), writes=[nm])
        op('dve', lambda e: e.tensor_tensor(out=o0[:], in0=o0[:], in1=o1[:], op=ALU.add), reads=['o0', 'o1'], writes=['o0'])
        op('dve', lambda e: e.reduce_sum(mean[:], o0[:], axis=AX.X), reads=['o0'], writes=['omean'])
        op('dve', lambda e: e.tensor_scalar(mean[:], mean[:], 1.0 / 64, None, op0=ALU.mult), reads=['omean'], writes=['omean'])
        op('dve', lambda e: e.tensor_tensor(out=o0[:], in0=o0[:], in1=mean[:].unsqueeze(2).to_broadcast([128, 8, 64]), op=ALU.subtract), reads=['o0', 'omean'], writes=['o0'])
        op('pool', lambda e: e.tensor_tensor(out=sq[:], in0=o0[:], in1=o0[:], op=ALU.mult), reads=['o0'], writes=['osq'])
        op('dve', lambda e: e.reduce_sum(var[:], sq[:], axis=AX.X), reads=['osq'], writes=['ovar'])
        op('dve', lambda e: e.tensor_scalar(var[:], var[:], 1.0 / 64, 64e-5, op0=ALU.mult, op1=ALU.add), reads=['ovar'], writes=['ovar'])
        op('act', lambda e: e.activation(var[:], var[:], AF.Sqrt), reads=['ovar'], writes=['ovar'])
        op('dve', lambda e: e.reciprocal(var[:], var[:]), reads=['ovar'], writes=['ovar'])
        op('dve', lambda e: e.tensor_tensor(out=o0[:], in0=o0[:], in1=var[:].unsqueeze(2).to_broadcast([128, 8, 64]), op=ALU.mult), reads=['o0', 'ovar'], writes=['o0'])
        op('dve', lambda e: e.tensor_tensor(out=o0[:], in0=o0[:], in1=v3(lng), op=ALU.mult), reads=['o0', 'lng'], writes=['o0'])
        op('pool', lambda e: e.tensor_tensor(out=o0[:], in0=o0[:], in1=v3(lnb), op=ALU.add), reads=['o0', 'lnb'], writes=['o0'])
        op('pool', lambda e: e.tensor_tensor(out=k0[:], in0=k0[:], in1=k1[:], op=ALU.add), reads=['ok0', 'ok1'], writes=['ok0'])
        op('pool', lambda e: e.tensor_tensor(out=k0[:], in0=k0[:], in1=rkb[:], op=ALU.mult), reads=['ok0', 'rkb'], writes=['ok0'])
        op('dve', lambda e: e.tensor_tensor(out=k0[:], in0=k0[:], in1=tr[:], op=ALU.mult), reads=['ok0', 'otr'], writes=['ok0'])
        op('dve', lambda e: e.reduce_sum(bon[:], v3(k0), axis=AX.X), reads=['ok0'], writes=['obon'])
        op('dve', lambda e: e.tensor_tensor(out=v3(tv), in0=v3(tv), in1=bon[:].unsqueeze(2).to_broadcast([128, 8, 64]), op=ALU.mult), reads=['otv', 'obon'], writes=['otv'])
        op('dve', lambda e: e.tensor_tensor(out=o0[:], in0=o0[:], in1=v3(tv), op=ALU.add), reads=['o0', 'otv'], writes=['o0'])
        op('dve', lambda e: e.tensor_tensor(out=o0[:], in0=o0[:], in1=v3(tg), op=ALU.mult), reads=['o0', 'otg'], writes=['o0'])
        for fb in range(4):
            P_ = pT[ct % 2]; pk_ = 'pTD%d' % (ct % 2); O = oT[ct % 2]; ok_ = 'oT%d' % (ct % 2); ct += 1
            op('pe', lambda e, P_=P_, fb=fb: e.transpose(P_[:], o0[:, 2 * fb:2 * fb + 2, :].rearrange("p a i -> p (a i)"), ident[:]), reads=['o0'], writes=[pk_])
            op('act', lambda e, P_=P_, O=O: e.copy(O[:], P_[:]), reads=[pk_], writes=[ok_])
            dma(lambda e, O=O, fb=fb, t0=t0: e.dma_start(out=Yd[fb * 128:(fb + 1) * 128, t0:t0 + 128], in_=O[:]), reads=[ok_])
    kb.end()


def _consts():
    c = {}
    c["c_ident"] = np.eye(128, dtype=np.float32)
    c["c_ones"] = np.ones((128, 128), np.float32)
    ps = np.zeros((128, 128), np.float32)
    for i in range(64):
        ps[2 * i, 2 * i + 1] = 1.0
        ps[2 * i + 1, 2 * i] = 1.0
    c["c_pswap"] = ps
    bo = np.zeros((128, 128), np.float32)
    bo[:64, :64] = 1.0
    bo[64:, 64:] = 1.0
    c["c_bones"] = bo
    sel = np.zeros((2, 256), np.float32)
    sel[0, 0:128] = 1.0
    sel[1, 128:256] = 1.0
    c["c_sel"] = sel
    t = np.arange(SEQ)
    r_idx = (t // 64).astype(np.float32)
    c_idx = (t % 64).astype(np.float32)

    def tables(rot_dim):
        n = rot_dim // 4
        inv = (10000.0 ** (-np.arange(n, dtype=np.float32) / n)).astype(np.float32)
        ang = np.concatenate([r_idx[:, None] * inv, c_idx[:, None] * inv], axis=-1).astype(np.float32)
        cos = np.cos(ang).astype(np.float32)
        sin = np.sin(ang).astype(np.float32)
        cosT = np.ones((rot_dim, TOK), np.float32)
        sinT = np.zeros((rot_dim, TOK), np.float32)
        for i in range(rot_dim // 2):
            cosT[2 * i, CTX:] = cos[:, i]
            cosT[2 * i + 1, CTX:] = cos[:, i]
            sinT[2 * i, CTX:] = -sin[:, i]
            sinT[2 * i + 1, CTX:] = sin[:, i]
        return cosT, sinT
    ca, sa = tables(32)
    cA = np.ones((128, TOK), np.float32); sA = np.zeros((128, TOK), np.float32)
    cA[64:96] = ca; sA[64:96] = sa
    c["c_cosA"] = cA; c["c_sinA"] = sA
    ch, sh = tables(64)
    c["c_cosH"] = np.concatenate([ch, ch], 0); c["c_sinH"] = np.concatenate([sh, sh], 0)
    k = np.arange(128)[:, None]; q = np.arange(128)[None, :]
    band = np.zeros((128, 256), np.float32)
    band[:, 0:128] = (q <= k)
    band[:, 128:256] = (k <= q)
    c["c_band"] = band
    c["c_iota16"] = np.tile(np.arange(16, dtype=np.float32)[None, :], (128, 1))
    hm = np.zeros((128, 512), np.float32)
    for r in range(128):
        hm[r, (r % 8) * 64:(r % 8) * 64 + 64] = 1.0
    c["c_hmask"] = hm
    return c


_CACHE = {}


def make_in_maps(inputs):
    consts = _consts()
    f = lambda a: np.ascontiguousarray(np.asarray(a, dtype=np.float32))
    shared = {k: f(inputs[k]) for k in ("ada_w", "ada_b", "w_in", "mla_q_norm", "mla_kv_norm", "mla_w_uq", "mla_w_ukv",
                                        "gqa_q_norm", "gqa_k_norm", "win_sink", "w_branch", "w_out", "ln1_g", "ln1_b", "ln2_g", "ln2_b",
                                        "peer_wq", "peer_keys", "peer_u", "peer_v", "rwkv_mu", "rwkv_w0", "rwkv_w2", "rwkv_a0", "rwkv_a2",
                                        "rwkv_g2", "rwkv_k_k", "rwkv_k_a", "rwkv_ln_g", "rwkv_ln_b")}
    shared["rwkv_r_k"] = f(inputs["rwkv_r_k"]).reshape(DEPTH, 512)
    shared.update(consts)
    maps = []
    for b in range(8):
        m = dict(shared)
        m["xin"] = np.ascontiguousarray(np.concatenate([f(inputs["ctx"][b]), f(inputs["x"][b])], axis=0))
        m["cc"] = np.ascontiguousarray(np.stack([f(inputs["c"][b]), f(inputs["c_ctx"])], axis=0))
        maps.append(m)
    return maps


def kernel(**inputs):
    kb = build()
    maps = make_in_maps(inputs)
    res = run_bass_kernel_spmd(kb.nc, maps, core_ids=list(range(8)))
    return np.stack([np.asarray(r["yout"], dtype=np.float32) for r in res.results], axis=0)
```

```python
import math
import numpy as np
import concourse.bass as bass
import concourse.mybir as mybir
from concourse.bass_utils import run_bass_kernel_spmd

F32 = mybir.dt.float32
I32 = mybir.dt.int32
U32 = mybir.dt.uint32
AF = mybir.ActivationFunctionType
ALU = mybir.AluOpType
AX = mybir.AxisListType

D = 1024
SEQ = 4096
CTX = 256
TOK = SEQ + CTX
NT = TOK // 128
DEPTH = 2
IN_W = 7968
OFF_MLA, OFF_RWKV, OFF_GQA, OFF_WIN, OFF_GATE = 0, 416, 2336, 3104, 3872
ALPHA = (2 * DEPTH) ** 0.25
MLA_SCALE = 96 ** -0.5
CHUNKS = [(0, 256)] + [(256 + 512 * i, 512) for i in range(8)]


class Prog:
    ENG = ('pe', 'act', 'dve', 'pool', 'sp')
    NDMA = 8

    def __init__(self, nc):
        self.nc = nc
        self.engobj = {'pe': nc.tensor, 'act': nc.scalar, 'dve': nc.vector, 'pool': nc.gpsimd, 'sp': nc.sync}
        self.sems, self.cnt, self.waited, self.lastw, self.readers = {}, {}, {}, {}, {}
        self.dma_n = {'sp': 0, 'pool': 0}
        self._stack = []
        self.nops = 0
        for e in self.ENG:
            self._mksem('done_' + e)
        for q in self.dma_n:
            for k in range(self.NDMA):
                self._mksem('dma_%s_%d' % (q, k))

    def _mksem(self, name):
        cm = self.nc.semaphore(name)
        self.sems[name] = cm.__enter__()
        self._stack.append(cm)
        self.cnt[name] = 0

    def _need(self, eng, tok, waits):
        if tok is None:
            return
        sname, val = tok
        if eng == 'pe' and sname == 'done_pe':
            return
        key = (eng, sname)
        if self.waited.get(key, 0) >= val:
            return
        self.waited[key] = val
        waits.append((sname, val))

    def _deps(self, eng, reads, writes):
        waits = []
        for b in reads:
            self._need(eng, self.lastw.get(b), waits)
        for b in writes:
            self._need(eng, self.lastw.get(b), waits)
            for t in self.readers.get(b, ()):
                self._need(eng, t, waits)
        return waits

    def _commit(self, tok, reads, writes):
        for b in reads:
            self.readers.setdefault(b, []).append(tok)
        for b in writes:
            self.lastw[b] = tok
            self.readers[b] = []

    def _emit(self, eng, waits, fn, inc):
        e = self.engobj[eng]
        for sname, val in waits:
            e.wait_ge(self.sems[sname], val)
        if fn is not None:
            fn(e).then_inc(self.sems[inc[0]], inc[1])
        self.nops += 1

    def op(self, eng, fn, reads=(), writes=()):
        waits = self._deps(eng, reads, writes)
        sname = 'done_' + eng
        self.cnt[sname] += 1
        tok = (sname, self.cnt[sname])
        self._emit(eng, waits, fn, (sname, 1))
        self._commit(tok, reads, writes)

    def dma(self, fn, reads=(), writes=(), q='sp'):
        waits = self._deps(q, reads, writes)
        n = self.dma_n[q]
        self.dma_n[q] += 1
        sname = 'dma_%s_%d' % (q, n % self.NDMA)
        if self.cnt[sname]:
            self._need(q, (sname, self.cnt[sname]), waits)
        self.cnt[sname] += 16
        tok = (sname, self.cnt[sname])
        self._emit(q, waits, fn, (sname, 16))
        self._commit(tok, reads, writes)

    def barrier(self, engs=None):
        for e in (engs or self.ENG):
            w = []
            for sname, c in self.cnt.items():
                if c:
                    self._need(e, (sname, c), w)
            self._emit(e, w, None, None)
        self.lastw.clear()
        self.readers.clear()


class KB:
    def __init__(self, dbg=()):
        self.nc = bass.Bass("TRN2", target_bir_lowering=False)
        self.p = Prog(self.nc)
        self.dbg = set(dbg)
        self.scopes = []
        self.uid = 0
        self.ins = {}

    def inp(self, name, shape, dt=F32):
        a = self.nc.dram_tensor(name, list(shape), dt, kind="ExternalInput").ap()
        self.ins[name] = a
        return a

    def scratch(self, name, shape, dt=F32, out=False):
        kind = "ExternalOutput" if (out or name in self.dbg) else "Internal"
        return self.nc.dram_tensor(name, list(shape), dt, kind=kind).ap()

    def begin(self):
        self.scopes.append([])

    def end(self):
        self.p.barrier()
        for cm in reversed(self.scopes.pop()):
            cm.__exit__(None, None, None)

    def sb(self, name, shape, dt=F32):
        self.uid += 1
        cm = self.nc.sbuf_tensor("%s_%d" % (name, self.uid), list(shape), dt)
        t = cm.__enter__()
        self.scopes[-1].append(cm)
        return t

    def ps(self, name, shape, dt=F32):
        self.uid += 1
        cm = self.nc.psum_tensor("%s_%d" % (name, self.uid), list(shape), dt)
        t = cm.__enter__()
        self.scopes[-1].append(cm)
        return t


def build(dbg=(), stop_after=None, nlayers=DEPTH, with_rwkv=True, rw_only=False, scan_steps=None, stages=None, rw_parts=None, peer_tiles=None, peer_slots=128, peer_upto=None):
    kb = KB(dbg)
    nc, p = kb.nc, kb.p
    op, dma = p.op, p.dma
    noop = lambda *a, **k: None

    def sub7(k):
        nonlocal op, dma
        if (peer_upto is None or k <= peer_upto) and (stages is None or 7 in stages):
            op, dma = p.op, p.dma
        else:
            op, dma = noop, noop

    def stage_on(n):
        nonlocal op, dma
        if stages is None or n in stages:
            op, dma = p.op, p.dma
        else:
            op, dma = noop, noop
    xin = kb.inp("xin", [TOK, D])
    cc = kb.inp("cc", [2, D])
    ada_w = kb.inp("ada_w", [DEPTH, D, 6 * D])
    ada_b = kb.inp("ada_b", [DEPTH, 6 * D])
    w_in = kb.inp("w_in", [DEPTH, D, IN_W])
    mla_q_norm = kb.inp("mla_q_norm", [DEPTH, 256])
    mla_kv_norm = kb.inp("mla_kv_norm", [DEPTH, 128])
    mla_w_uq = kb.inp("mla_w_uq", [DEPTH, 256, 768])
    mla_w_ukv = kb.inp("mla_w_ukv", [DEPTH, 128, 1024])
    gqa_q_norm = kb.inp("gqa_q_norm", [DEPTH, 64])
    gqa_k_norm = kb.inp("gqa_k_norm", [DEPTH, 64])
    win_sink = kb.inp("win_sink", [DEPTH, 8])
    w_branch = kb.inp("w_branch", [DEPTH, 4, 512, D])
    w_out = kb.inp("w_out", [DEPTH, D, D])
    ln1_g = kb.inp("ln1_g", [DEPTH, D]); ln1_b = kb.inp("ln1_b", [DEPTH, D])
    ln2_g = kb.inp("ln2_g", [DEPTH, D]); ln2_b = kb.inp("ln2_b", [DEPTH, D])
    peer_wq = kb.inp("peer_wq", [DEPTH, D, D])
    peer_keys = kb.inp("peer_keys", [DEPTH, 8, 2, 128, 64])
    peer_u = kb.inp("peer_u", [DEPTH, 16384, D])
    peer_v = kb.inp("peer_v", [DEPTH, 16384, D])
    rw = {}
    for nm, shp in [("rwkv_mu", [DEPTH, 2, 1920]), ("rwkv_w0", [DEPTH, 2, 512]), ("rwkv_w2", [DEPTH, 2, 64, 512]),
                    ("rwkv_a0", [DEPTH, 2, 512]), ("rwkv_a2", [DEPTH, 2, 64, 512]), ("rwkv_g2", [DEPTH, 128, 512]),
                    ("rwkv_k_k", [DEPTH, 512]), ("rwkv_k_a", [DEPTH, 512]), ("rwkv_r_k", [DEPTH, 512]),
                    ("rwkv_ln_g", [DEPTH, 512]), ("rwkv_ln_b", [DEPTH, 512])]:
        rw[nm] = kb.inp(nm, shp)
    c_ident = kb.inp("c_ident", [128, 128])
    c_ones = kb.inp("c_ones", [128, 128])
    c_pswap = kb.inp("c_pswap", [128, 128])
    c_bones = kb.inp("c_bones", [128, 128])
    c_sel = kb.inp("c_sel", [2, 256])
    c_cosA = kb.inp("c_cosA", [128, TOK]); c_sinA = kb.inp("c_sinA", [128, TOK])
    c_cosH = kb.inp("c_cosH", [128, TOK]); c_sinH = kb.inp("c_sinH", [128, TOK])
    c_band = kb.inp("c_band", [128, 256])
    c_iota16 = kb.inp("c_iota16", [128, 16])
    c_hmask = kb.inp("c_hmask", [128, 512])
    yout = kb.scratch("yout", [SEQ, D], out=True)
    XS1 = kb.scratch("XS1", [TOK, D])
    XMID = kb.scratch("XMID", [TOK, D])
    PT = kb.scratch("PT", [IN_W, TOK])
    QA = kb.scratch("QA", [8 * 96, TOK]); KA = kb.scratch("KA", [8 * 96, TOK]); VA = kb.scratch("VA", [TOK, 8, 65])
    QC = kb.scratch("QC", [512, TOK]); KC = kb.scratch("KC", [128, TOK]); VC = kb.scratch("VC", [TOK, 2, 65])
    QD = kb.scratch("QD", [512, TOK]); KD = kb.scratch("KD", [128, TOK]); VD = kb.scratch("VD", [TOK, 2, 65])
    YT = [kb.scratch("YT%d" % i, [512, TOK]) for i in range(4)]

    kb.begin()
    ident = kb.sb("ident", [128, 128]); ones = kb.sb("ones", [128, 128]); pswap = kb.sb("pswap", [128, 128])
    bones = kb.sb("bones", [128, 128]); sel = kb.sb("sel", [2, 256])
    for t, src in [(ident, c_ident), (ones, c_ones), (pswap, c_pswap), (bones, c_bones), (sel, c_sel)]:
        dma(lambda e, t=t, src=src: e.dma_start(out=t[:], in_=src))
    p.barrier()

    for l in range(nlayers):
        last = (l == DEPTH - 1)
        XS = xin if l == 0 else XS1
        XN = XS1
        kb.begin()
        modT = kb.sb("modT", [128, 48, 2])
        A1 = kb.sb("A1", [128, 8, 2])
        bc = {nm: [kb.sb("bc_%s%d" % (nm, w), [128, D]) for w in range(2)] for nm in ("gt1", "sh2", "sc2", "gt2")}
        lnp = {nm: kb.sb("ln_" + nm, [128, D]) for nm in ("g1", "b1", "g2", "b2")}
        if rw_only:
            rwkv_stage(kb, l, PT, YT[1], rw, dict(ident=ident, ones=ones, bones=bones, c_hmask=c_hmask), last, scan_steps)
            kb.end()
            break
        stage_on(0)
        kb.begin()
        scT = kb.sb("scT", [128, 8, 2]); modrow = kb.sb("modrow", [2, 6 * D]); bias2 = kb.sb("bias2", [2, 6 * D])
        wb = [kb.sb("adaw%d" % i, [128, 8, 512]) for i in range(2)]
        pm = [kb.ps("pm%d" % i, [128, 512]) for i in range(2)]
        pt = kb.ps("ptm", [128, 96])
        for r in range(2):
            dma(lambda e, r=r: e.dma_start(out=scT[:, :, r], in_=cc[r].rearrange("(k p) -> p k", p=128), allow_slow_non_contiguous=True), writes=['scT'])
        for r in range(2):
            dma(lambda e, r=r: e.dma_start(out=bias2[r:r + 1, :], in_=ada_b[l:l + 1, :]), writes=['bias2'])
        for (t, src) in [(lnp["g1"], ln1_g), (lnp["b1"], ln1_b), (lnp["g2"], ln2_g), (lnp["b2"], ln2_b)]:
            dma(lambda e, t=t, src=src: e.dma_start(out=t[:], in_=src[l:l + 1, :].partition_broadcast(128) if False else src[l, :].partition_broadcast(128)))
        op('act', lambda e: e.activation(scT[:], scT[:], AF.Silu), reads=['scT'], writes=['scT'])
        for g in range(12):
            W = wb[g % 2]; k_ = 'adaw%d' % (g % 2); P_ = pm[g % 2]; pk_ = 'pm%d' % (g % 2)
            dma(lambda e, W=W, g=g: e.dma_start(out=W[:], in_=ada_w[l, :, g * 512:(g + 1) * 512].rearrange("(k p) n -> p k n", p=128)), writes=[k_])
            for k in range(8):
                op('pe', lambda e, W=W, P_=P_, k=k: e.matmul(P_[0:2, :], scT[:, k, :], W[:, k, :], start=(k == 0), stop=(k == 7)),
                   reads=['scT', k_], writes=[pk_])
            op('dve', lambda e, P_=P_, g=g: e.tensor_tensor(out=modrow[:, g * 512:(g + 1) * 512], in0=P_[0:2, :], in1=bias2[:, g * 512:(g + 1) * 512], op=ALU.add),
               reads=[pk_, 'bias2'], writes=['modrow'])
        for c in range(48):
            op('pe', lambda e, c=c: e.transpose(pt[:, 2 * c:2 * c + 2], modrow[0:2, c * 128:(c + 1) * 128], ident[0:2, 0:2]),
               reads=['modrow'], writes=['ptm'])
        op('dve', lambda e: e.tensor_copy(modT[:].rearrange("p c t -> p (c t)"), pt[:]), reads=['ptm'], writes=['modT'])
        op('dve', lambda e: e.tensor_scalar_add(A1[:], modT[:, 8:16, :], 1.0), reads=['modT'], writes=['A1'])
        for nm, ci in (("gt1", 2), ("sh2", 3), ("sc2", 4), ("gt2", 5)):
            for w in range(2):
                for hf in range(2):
                    P_ = pm[hf]; pk_ = 'pm%d' % hf
                    op('pe', lambda e, P_=P_, w=w, ci=ci, hf=hf: e.matmul(P_[:], sel[:, (1 - w) * 0 + w * 128:(w + 1) * 128] if False else sel[:, w * 128:(w + 1) * 128],
                                                                       modrow[:, ci * D + hf * 512: ci * D + (hf + 1) * 512], start=True, stop=True),
                       reads=['modrow'], writes=[pk_])
                    if nm == "sc2":
                        op('act', lambda e, P_=P_, nm=nm, w=w, hf=hf: e.activation(bc[nm][w][:, hf * 512:(hf + 1) * 512], P_[:], AF.Identity, bias=1.0),
                           reads=[pk_], writes=['bc'])
                    else:
                        op('act', lambda e, P_=P_, nm=nm, w=w, hf=hf: e.activation(bc[nm][w][:, hf * 512:(hf + 1) * 512], P_[:], AF.Identity),
                           reads=[pk_], writes=['bc'])
        kb.end()
        if stop_after == 's0':
            break
        stage_on(1)
        kb.begin()
        xmT = kb.sb("xmT", [128, 8, 512])
        xb = [kb.sb("xb%d" % i, [128, D]) for i in range(2)]
        wb = [kb.sb("winb%d" % i, [128, 8, 512]) for i in range(2)]
        ob = [kb.sb("ob%d" % i, [128, 512]) for i in range(3)]
        ptx = [kb.ps("ptx%d" % i, [128, 128]) for i in range(2)]
        pp = [kb.ps("pp%d" % i, [128, 512]) for i in range(3)]
        cnt = 0
        for ci, (n0, n) in enumerate(CHUNKS):
            which = 1 if ci == 0 else 0
            for sub in range(n // 128):
                t = (n0 // 128) + sub
                X = xb[t % 2]; xk = 'xb%d' % (t % 2)
                dma(lambda e, X=X, t=t: e.dma_start(out=X[:], in_=XS[t * 128:(t + 1) * 128, :]), writes=[xk])
                for k in range(8):
                    P_ = ptx[k % 2]; pk_ = 'ptx%d' % (k % 2)
                    op('pe', lambda e, P_=P_, X=X, k=k: e.transpose(P_[:], X[:, k * 128:(k + 1) * 128], ident[:]), reads=[xk], writes=[pk_])
                    op('act', lambda e, P_=P_, k=k, sub=sub, which=which: e.activation(
                        xmT[:, k, sub * 128:(sub + 1) * 128], P_[:], AF.Identity, scale=A1[:, k, which:which + 1], bias=modT[:, k, which:which + 1]),
                       reads=[pk_], writes=['xmT'])
            for g in range(16):
                c0 = g * 512; ncol = min(512, IN_W - c0)
                W = wb[g % 2]; wk_ = 'winb%d' % (g % 2)
                dma(lambda e, W=W, c0=c0, ncol=ncol: e.dma_start(out=W[:, :, 0:ncol], in_=w_in[l, :, c0:c0 + ncol].rearrange("(k p) n -> p k n", p=128)), writes=[wk_])
                for fb in range((ncol + 127) // 128):
                    m = min(128, ncol - fb * 128)
                    P_ = pp[cnt % 3]; pk_ = 'pp%d' % (cnt % 3); O = ob[cnt % 3]; ok_ = 'ob%d' % (cnt % 3)
                    for k in range(8):
                        op('pe', lambda e, P_=P_, W=W, k=k, fb=fb, m=m, n=n: e.matmul(P_[0:m, 0:n], W[:, k, fb * 128:fb * 128 + m], xmT[:, k, 0:n], start=(k == 0), stop=(k == 7)),
                           reads=[wk_, 'xmT'], writes=[pk_])
                    eng = 'act' if cnt % 2 == 0 else 'dve'
                    if eng == 'act':
                        op('act', lambda e, O=O, P_=P_, m=m, n=n: e.copy(O[0:m, 0:n], P_[0:m, 0:n]), reads=[pk_], writes=[ok_])
                    else:
                        op('dve', lambda e, O=O, P_=P_, m=m, n=n: e.tensor_copy(O[0:m, 0:n], P_[0:m, 0:n]), reads=[pk_], writes=[ok_])
                    r0 = c0 + fb * 128
                    dma(lambda e, O=O, r0=r0, m=m, n0=n0, n=n: e.dma_start(out=PT[r0:r0 + m, n0:n0 + n], in_=O[0:m, 0:n]), reads=[ok_])
                    cnt += 1
        kb.end()
        if stop_after == 's1':
            break
        stage_on(2)
        kb.begin()
        wuq = kb.sb("wuq", [128, 2, 768]); wukv = kb.sb("wukv", [128, 1024])
        gq = kb.sb("gq", [128, 2]); gkv = kb.sb("gkv", [128, 1])
        dq = kb.sb("dq", [128, 2, 512]); dkv = kb.sb("dkv", [128, 512]); kr = kb.sb("kr", [128, 512]); krr = kb.sb("krr", [128, 512])
        sq = kb.sb("sq", [128, 3, 512]); rq = kb.sb("rq", [128, 512]); rkv = kb.sb("rkv", [128, 512])
        cosA = kb.sb("cosA", [128, 512]); sinA = kb.sb("sinA", [128, 512])
        t1 = kb.sb("t1", [128, 512]); t2 = kb.sb("t2", [128, 512])
        qh = [kb.sb("qh%d" % i, [128, 512]) for i in range(2)]
        kh = [kb.sb("kh%d" % i, [128, 512]) for i in range(2)]
        va = [kb.sb("va%d" % i, [128, 8, 65]) for i in range(2)]
        rt = kb.sb("rt", [128, 1])
        pss = [kb.ps("pss%d" % i, [128, 512]) for i in range(2)]
        pq = kb.ps("pq", [128, 512]); pk = kb.ps("pk", [128, 512]); py = kb.ps("py", [128, 512])
        pv = kb.ps("pv", [128, 1024]); pr = kb.ps("pr", [128, 8])
        dma(lambda e: e.dma_start(out=wuq[:], in_=mla_w_uq[l].rearrange("(k p) n -> p k n", p=128)), writes=['wuq'])
        dma(lambda e: e.dma_start(out=wukv[:], in_=mla_w_ukv[l]), writes=['wukv'])
        dma(lambda e: e.dma_start(out=gq[:], in_=mla_q_norm[l].rearrange("(k p) -> p k", p=128), allow_slow_non_contiguous=True), writes=['gq'])
        dma(lambda e: e.dma_start(out=gkv[:], in_=mla_kv_norm[l].rearrange("(p o) -> p o", o=1)), writes=['gkv'])
        for i in range(2):
            op('pool', lambda e, i=i: e.memset(va[i][:], 1.0), writes=['va%d' % i])
        hc = 0
        for ci, (n0, n) in enumerate(CHUNKS):
            dma(lambda e, n0=n0, n=n: e.dma_start(out=dq[:, :, 0:n], in_=PT[0:256, n0:n0 + n].rearrange("(k p) n -> p k n", p=128)), writes=['dq'])
            dma(lambda e, n0=n0, n=n: e.dma_start(out=dkv[:, 0:n], in_=PT[256:384, n0:n0 + n]), writes=['dkv'])
            dma(lambda e, n0=n0, n=n: e.dma_start(out=kr[64:96, 0:n], in_=PT[384:416, n0:n0 + n]), writes=['kr'])
            dma(lambda e, n0=n0, n=n: e.dma_start(out=cosA[64:96, 0:n], in_=c_cosA[64:96, n0:n0 + n]), writes=['cosA'])
            dma(lambda e, n0=n0, n=n: e.dma_start(out=sinA[64:96, 0:n], in_=c_sinA[64:96, n0:n0 + n]), writes=['sinA'])
            op('act', lambda e, n=n: e.activation(sq[:, 0:2, 0:n], dq[:, :, 0:n], AF.Square), reads=['dq'], writes=['sq'])
            op('act', lambda e, n=n: e.activation(sq[:, 2, 0:n], dkv[:, 0:n], AF.Square), reads=['dkv'], writes=['sq'])
            for k in range(2):
                op('pe', lambda e, k=k, n=n: e.matmul(pss[0][:, 0:n], ones[:], sq[:, k, 0:n], start=(k == 0), stop=(k == 1)), reads=['sq'], writes=['pss0'])
            op('pe', lambda e, n=n: e.matmul(pss[1][:, 0:n], ones[:], sq[:, 2, 0:n], start=True, stop=True), reads=['sq'], writes=['pss1'])
            op('dve', lambda e, n=n: e.tensor_scalar(rq[:, 0:n], pss[0][:, 0:n], 1.0 / 256, 1e-6, op0=ALU.mult, op1=ALU.add), reads=['pss0'], writes=['rq'])
            op('act', lambda e, n=n: e.activation(rq[:, 0:n], rq[:, 0:n], AF.Sqrt), reads=['rq'], writes=['rq'])
            op('dve', lambda e, n=n: e.reciprocal(rq[:, 0:n], rq[:, 0:n]), reads=['rq'], writes=['rq'])
            op('dve', lambda e, n=n: e.tensor_scalar(rkv[:, 0:n], pss[1][:, 0:n], 1.0 / 128, 1e-6, op0=ALU.mult, op1=ALU.add), reads=['pss1'], writes=['rkv'])
            op('act', lambda e, n=n: e.activation(rkv[:, 0:n], rkv[:, 0:n], AF.Sqrt), reads=['rkv'], writes=['rkv'])
            op('dve', lambda e, n=n: e.reciprocal(rkv[:, 0:n], rkv[:, 0:n]), reads=['rkv'], writes=['rkv'])
            for k in range(2):
                op('act', lambda e, k=k, n=n: e.mul(dq[:, k, 0:n], dq[:, k, 0:n], gq[:, k:k + 1]), reads=['dq', 'gq'], writes=['dq'])
            op('act', lambda e, n=n: e.mul(dkv[:, 0:n], dkv[:, 0:n], gkv[:, 0:1]), reads=['dkv', 'gkv'], writes=['dkv'])
            op('pe', lambda e, n=n: e.matmul(py[64:96, 0:n], pswap[64:96, 64:96], kr[64:96, 0:n], start=True, stop=True), reads=['kr'], writes=['py'])
            op('dve', lambda e, n=n: e.tensor_tensor(out=t1[64:96, 0:n], in0=kr[64:96, 0:n], in1=cosA[64:96, 0:n], op=ALU.mult), reads=['kr', 'cosA'], writes=['t1'])
            op('dve', lambda e, n=n: e.tensor_tensor(out=t2[64:96, 0:n], in0=py[64:96, 0:n], in1=sinA[64:96, 0:n], op=ALU.mult), reads=['py', 'sinA'], writes=['t2'])
            op('dve', lambda e, n=n: e.tensor_tensor(out=krr[64:96, 0:n], in0=t1[64:96, 0:n], in1=t2[64:96, 0:n], op=ALU.add), reads=['t1', 't2'], writes=['krr'])
            for h in range(8):
                Q = qh[hc % 2]; qk_ = 'qh%d' % (hc % 2); K_ = kh[hc % 2]; kk_ = 'kh%d' % (hc % 2)
                hc += 1
                for k in range(2):
                    op('pe', lambda e, k=k, h=h, n=n: e.matmul(pq[0:96, 0:n], wuq[:, k, h * 96:(h + 1) * 96], dq[:, k, 0:n], start=(k == 0), stop=(k == 1)),
                       reads=['wuq', 'dq'], writes=['pq'])
                op('dve', lambda e, Q=Q, n=n: e.scalar_tensor_tensor(out=Q[0:96, 0:n], in0=pq[0:96, 0:n], scalar=MLA_SCALE, in1=rq[0:96, 0:n], op0=ALU.mult, op1=ALU.mult),
                   reads=['pq', 'rq'], writes=[qk_])
                op('pe', lambda e, Q=Q, n=n: e.matmul(py[64:96, 0:n], pswap[64:96, 64:96], Q[64:96, 0:n], start=True, stop=True), reads=[qk_], writes=['py'])
                op('dve', lambda e, Q=Q, n=n: e.tensor_tensor(out=t1[64:96, 0:n], in0=Q[64:96, 0:n], in1=cosA[64:96, 0:n], op=ALU.mult), reads=[qk_, 'cosA'], writes=['t1'])
                op('dve', lambda e, n=n: e.tensor_tensor(out=t2[64:96, 0:n], in0=py[64:96, 0:n], in1=sinA[64:96, 0:n], op=ALU.mult), reads=['py', 'sinA'], writes=['t2'])
                op('dve', lambda e, Q=Q, n=n: e.tensor_tensor(out=Q[64:96, 0:n], in0=t1[64:96, 0:n], in1=t2[64:96, 0:n], op=ALU.add), reads=['t1', 't2'], writes=[qk_])
                dma(lambda e, Q=Q, h=h, n0=n0, n=n: e.dma_start(out=QA[h * 96:(h + 1) * 96, n0:n0 + n], in_=Q[0:96, 0:n]), reads=[qk_])
                op('pe', lambda e, h=h, n=n: e.matmul(pk[0:64, 0:n], wukv[:, h * 128:h * 128 + 64], dkv[:, 0:n], start=True, stop=True), reads=['wukv', 'dkv'], writes=['pk'])
                op('dve', lambda e, K_=K_, n=n: e.tensor_tensor(out=K_[0:64, 0:n], in0=pk[0:64, 0:n], in1=rkv[0:64, 0:n], op=ALU.mult), reads=['pk', 'rkv'], writes=[kk_])
                op('pool', lambda e, K_=K_, n=n: e.tensor_copy(K_[64:96, 0:n], krr[64:96, 0:n]), reads=['krr'], writes=[kk_])
                dma(lambda e, K_=K_, h=h, n0=n0, n=n: e.dma_start(out=KA[h * 96:(h + 1) * 96, n0:n0 + n], in_=K_[0:96, 0:n]), reads=[kk_])
            for sub in range(n // 128):
                t = n0 // 128 + sub
                V_ = va[t % 2]; vk_ = 'va%d' % (t % 2)
                op('pe', lambda e, sub=sub: e.matmul(pr[:, 0:1], sq[:, 2, sub * 128:(sub + 1) * 128], ones[:, 0:1], start=True, stop=True), reads=['sq'], writes=['pr'])
                op('dve', lambda e: e.tensor_scalar(rt[:], pr[:, 0:1], 1.0 / 128, 1e-6, op0=ALU.mult, op1=ALU.add), reads=['pr'], writes=['rt'])
                op('act', lambda e: e.activation(rt[:], rt[:], AF.Sqrt), reads=['rt'], writes=['rt'])
                op('dve', lambda e: e.reciprocal(rt[:], rt[:]), reads=['rt'], writes=['rt'])
                for hf in range(2):
                    op('pe', lambda e, sub=sub, hf=hf: e.matmul(pv[:, hf * 512:(hf + 1) * 512], dkv[:, sub * 128:(sub + 1) * 128], wukv[:, hf * 512:(hf + 1) * 512], start=True, stop=True),
                       reads=['dkv', 'wukv'], writes=['pv'])
                op('act', lambda e, V_=V_: e.activation(V_[:, :, 0:64], pv[:].rearrange("p (h c) -> p h c", c=128)[:, :, 64:128], AF.Copy, scale=rt[:, 0:1]),
                   reads=['pv', 'rt'], writes=[vk_])
                dma(lambda e, V_=V_, t=t: e.dma_start(out=VA[t * 128:(t + 1) * 128, :, :], in_=V_[:]), reads=[vk_])
        kb.end()
        if stop_after == 's2':
            break
        stage_on(3)
        kb.begin()
        cosH = kb.sb("cosH", [128, 512]); sinH = kb.sb("sinH", [128, 512])
        gn = kb.sb("gn", [128, 2])
        X3 = [kb.sb("x3_%d" % i, [128, 512]) for i in range(2)]
        sq3 = kb.sb("sq3", [128, 512]); r3 = kb.sb("r3", [128, 512]); t1 = kb.sb("t1b", [128, 512]); t2 = kb.sb("t2b", [128, 512])
        O3 = [kb.sb("o3_%d" % i, [128, 512]) for i in range(2)]
        vc = [kb.sb("vc%d" % i, [128, 2, 65]) for i in range(2)]
        pss3 = kb.ps("pss3", [128, 512]); py3 = kb.ps("py3", [128, 512]); pt3 = kb.ps("pt3", [128, 128])
        for j, src in enumerate((gqa_q_norm, gqa_k_norm)):
            for hh in range(2):
                dma(lambda e, j=j, src=src, hh=hh: e.dma_start(out=gn[hh * 64:(hh + 1) * 64, j:j + 1], in_=src[l].rearrange("(p o) -> p o", o=1)), writes=['gn'])
        for i in range(2):
            op('pool', lambda e, i=i: e.memset(vc[i][:], 1.0), writes=['vc%d' % i])
        xc = 0
        for (base, norm, Qd, Kd, Vd) in ((OFF_GQA, True, QC, KC, VC), (OFF_WIN, False, QD, KD, VD)):
            for ci, (n0, n) in enumerate(CHUNKS):
                dma(lambda e, n0=n0, n=n: e.dma_start(out=cosH[:, 0:n], in_=c_cosH[:, n0:n0 + n]), writes=['cosH'])
                dma(lambda e, n0=n0, n=n: e.dma_start(out=sinH[:, 0:n], in_=c_sinH[:, n0:n0 + n]), writes=['sinH'])
                for fb in range(5):
                    X = X3[xc % 2]; xk = 'x3_%d' % (xc % 2); O = O3[xc % 2]; ok_ = 'o3_%d' % (xc % 2)
                    xc += 1
                    dma(lambda e, X=X, base=base, fb=fb, n0=n0, n=n: e.dma_start(out=X[:, 0:n], in_=PT[base + fb * 128: base + (fb + 1) * 128, n0:n0 + n]), writes=[xk])
                    if norm:
                        gcol = 0 if fb < 4 else 1
                        op('act', lambda e, X=X, n=n: e.activation(sq3[:, 0:n], X[:, 0:n], AF.Square), reads=[xk], writes=['sq3'])
                        op('pe', lambda e, n=n: e.matmul(pss3[:, 0:n], bones[:], sq3[:, 0:n], start=True, stop=True), reads=['sq3'], writes=['pss3'])
                        op('dve', lambda e, n=n: e.tensor_scalar(r3[:, 0:n], pss3[:, 0:n], 1.0 / 64, 1e-6, op0=ALU.mult, op1=ALU.add), reads=['pss3'], writes=['r3'])
                        op('act', lambda e, n=n: e.activation(r3[:, 0:n], r3[:, 0:n], AF.Sqrt), reads=['r3'], writes=['r3'])
                        op('dve', lambda e, n=n: e.reciprocal(r3[:, 0:n], r3[:, 0:n]), reads=['r3'], writes=['r3'])
                        op('dve', lambda e, X=X, n=n, gcol=gcol: e.scalar_tensor_tensor(out=X[:, 0:n], in0=X[:, 0:n], scalar=gn[:, gcol:gcol + 1], in1=r3[:, 0:n], op0=ALU.mult, op1=ALU.mult),
                           reads=[xk, 'r3', 'gn'], writes=[xk])
                    op('pe', lambda e, X=X, n=n: e.matmul(py3[:, 0:n], pswap[:], X[:, 0:n], start=True, stop=True), reads=[xk], writes=['py3'])
                    op('pool', lambda e, X=X, n=n: e.tensor_tensor(out=t1[:, 0:n], in0=X[:, 0:n], in1=cosH[:, 0:n], op=ALU.mult), reads=[xk, 'cosH'], writes=['t1b'])
                    op('dve', lambda e, n=n: e.tensor_tensor(out=t2[:, 0:n], in0=py3[:, 0:n], in1=sinH[:, 0:n], op=ALU.mult), reads=['py3', 'sinH'], writes=['t2b'])
                    op('dve', lambda e, O=O, n=n: e.tensor_tensor(out=O[:, 0:n], in0=t1[:, 0:n], in1=t2[:, 0:n], op=ALU.add), reads=['t1b', 't2b'], writes=[ok_])
                    if fb < 4:
                        dma(lambda e, O=O, Qd=Qd, fb=fb, n0=n0, n=n: e.dma_start(out=Qd[fb * 128:(fb + 1) * 128, n0:n0 + n], in_=O[:, 0:n]), reads=[ok_])
                    else:
                        dma(lambda e, O=O, Kd=Kd, n0=n0, n=n: e.dma_start(out=Kd[:, n0:n0 + n], in_=O[:, 0:n]), reads=[ok_])
                X = X3[xc % 2]; xk = 'x3_%d' % (xc % 2)
                xc += 1
                dma(lambda e, X=X, base=base, n0=n0, n=n: e.dma_start(out=X[:, 0:n], in_=PT[base + 640: base + 768, n0:n0 + n]), writes=[xk])
                for sub in range(n // 128):
                    t = n0 // 128 + sub
                    V_ = vc[t % 2]; vk_ = 'vc%d' % (t % 2)
                    op('pe', lambda e, X=X, sub=sub: e.transpose(pt3[:], X[:, sub * 128:(sub + 1) * 128], ident[:]), reads=[xk], writes=['pt3'])
                    op('act', lambda e, V_=V_: e.activation(V_[:, :, 0:64], pt3[:].rearrange("p (h c) -> p h c", c=64), AF.Copy), reads=['pt3'], writes=[vk_])
                    dma(lambda e, V_=V_, Vd=Vd, t=t: e.dma_start(out=Vd[t * 128:(t + 1) * 128, :, :], in_=V_[:]), reads=[vk_])
        kb.end()
        if stop_after == 's3':
            break
        stage_on(4)
        qtiles_ctx = [] if last else [0, 1]

        def attention(Qd, Kd, Vd, dq_, nkv, G, scale, mode, Yd, sinkp):
            kb.begin()
            Kh = kb.sb("Kh", [128, TOK]); Vh = kb.sb("Vh", [128, NT, 65])
            Qh = [kb.sb("Qh%d" % i, [128, 512]) for i in range(2)]
            PtAll = kb.sb("PtAll", [128, NT, 256])
            Oall = kb.sb("Oall", [128, NT, 512])
            rec = kb.sb("rec", [128, 4]); den = kb.sb("den", [128, 4])
            band = kb.sb("band", [128, 256]); esink = kb.sb("esink", [128, 8])
            ot = [kb.sb("ot%d" % i, [128, 128]) for i in range(2)]
            psS = [kb.ps("psS%d" % i, [128, 512]) for i in range(3)]
            po = [kb.ps("po%d" % i, [128, 128]) for i in range(2)]
            ptt = [kb.ps("ptt%d" % i, [128, 128]) for i in range(2)]
            dma(lambda e: e.dma_start(out=band[:], in_=c_band), writes=['band'])
            if sinkp:
                dma(lambda e: e.dma_start(out=esink[:], in_=win_sink[l, :].partition_broadcast(128)), writes=['esink'])
                op('act', lambda e: e.activation(esink[:], esink[:], AF.Exp), reads=['esink'], writes=['esink'])
            groups = []
            if qtiles_ctx:
                groups.append(([0, 1], [(0, None), (1, None)], True))
            if mode == 'full':
                for c in range(16):
                    groups.append(([2 + 2 * c, 3 + 2 * c], [(kt, None) for kt in range(NT)], False))
            else:
                for t in range(2, NT):
                    ks = [(0, None), (1, None)]
                    if t - 1 >= 2:
                        ks.append((t - 1, 0))
                    ks.append((t, None))
                    if t + 1 < NT:
                        ks.append((t + 1, 1))
                    groups.append(([t], ks, False))
            cq = 0; cp = 0; ca = 0
            for kvh in range(nkv):
                dma(lambda e, kvh=kvh: e.dma_start(out=Kh[0:dq_, :], in_=Kd[kvh * dq_:(kvh + 1) * dq_, :]), writes=['Kh'])
                dma(lambda e, kvh=kvh: e.dma_start(out=Vh[:], in_=Vd[:, kvh, :].rearrange("(t p) c -> p t c", p=128)), writes=['Vh'])
                for (qts, ks, isctx) in groups:
                    n = 128 * len(qts); q0 = qts[0] * 128
                    for g in range(G):
                        h = kvh * G + g
                        Q = Qh[cq % 2]; qk_ = 'Qh%d' % (cq % 2); cq += 1
                        dma(lambda e, Q=Q, h=h, q0=q0, n=n: e.dma_start(out=Q[0:dq_, 0:n], in_=Qd[h * dq_:(h + 1) * dq_, q0:q0 + n]), writes=[qk_])
                        for ki, (kt, mk) in enumerate(ks):
                            S = psS[cp % 3]; sk_ = 'psS%d' % (cp % 3); cp += 1
                            pk_ = 'PtAll%d' % ki
                            op('pe', lambda e, S=S, Q=Q, kt=kt, n=n: e.matmul(S[:, 0:n], Kh[0:dq_, kt * 128:(kt + 1) * 128], Q[0:dq_, 0:n], start=True, stop=True),
                               reads=['Kh', qk_], writes=[sk_])
                            op('act', lambda e, S=S, ki=ki, n=n: e.activation(PtAll[:, ki, 0:n], S[:, 0:n], AF.Exp, scale=scale), reads=[sk_], writes=[pk_])
                            if mk is not None:
                                op('pool', lambda e, ki=ki, mk=mk: e.tensor_tensor(out=PtAll[:, ki, 0:128], in0=PtAll[:, ki, 0:128], in1=band[:, mk * 128:(mk + 1) * 128], op=ALU.mult),
                                   reads=[pk_, 'band'], writes=[pk_])
                        for sub in range(len(qts)):
                            PO = po[ca % 2]; pok = 'po%d' % (ca % 2); ca += 1
                            for ki, (kt, mk) in enumerate(ks):
                                op('pe', lambda e, PO=PO, sub=sub, kt=kt, ki=ki, nk=len(ks): e.matmul(
                                    PO[:, 0:65], PtAll[:, ki, sub * 128:(sub + 1) * 128], Vh[:, kt, :], start=(ki == 0), stop=(ki == nk - 1)),
                                   reads=['PtAll%d' % ki, 'Vh'], writes=[pok])
                            if sinkp:
                                op('dve', lambda e, PO=PO, h=h: e.tensor_scalar(den[:, 0:1], PO[:, 64:65], esink[:, h:h + 1], None, op0=ALU.add), reads=[pok, 'esink'], writes=['den'])
                                op('dve', lambda e: e.reciprocal(rec[:, 0:1], den[:, 0:1]), reads=['den'], writes=['rec'])
                            else:
                                op('dve', lambda e, PO=PO: e.reciprocal(rec[:, 0:1], PO[:, 64:65]), reads=[pok], writes=['rec'])
                            op('dve', lambda e, PO=PO, t=qts[sub], h=h: e.tensor_scalar(Oall[:, t, h * 64:(h + 1) * 64], PO[:, 0:64], rec[:, 0:1], None, op0=ALU.mult),
                               reads=[pok, 'rec'], writes=['Oall'])
            ct = 0
            for t in (qtiles_ctx + list(range(2, NT))):
                for fb in range(4):
                    P_ = ptt[ct % 2]; pk_ = 'ptt%d' % (ct % 2); O = ot[ct % 2]; ok_ = 'ot%d' % (ct % 2); ct += 1
                    op('pe', lambda e, P_=P_, t=t, fb=fb: e.transpose(P_[:], Oall[:, t, fb * 128:(fb + 1) * 128], ident[:]), reads=['Oall'], writes=[pk_])
                    op('act', lambda e, P_=P_, O=O: e.copy(O[:], P_[:]), reads=[pk_], writes=[ok_])
                    dma(lambda e, O=O, t=t, fb=fb: e.dma_start(out=Yd[fb * 128:(fb + 1) * 128, t * 128:(t + 1) * 128], in_=O[:]), reads=[ok_])
            kb.end()

        attention(QA, KA, VA, 96, 8, 1, 1.0, 'full', YT[0], False)
        attention(QC, KC, VC, 64, 2, 4, 0.125, 'full', YT[2], False)
        attention(QD, KD, VD, 64, 2, 4, 0.125, 'win', YT[3], True)
        if stop_after == 's4':
            break
        if with_rwkv and (stages is None or 5 in stages):
            rwkv_stage(kb, l, PT, YT[1], rw, dict(ident=ident, ones=ones, bones=bones, c_hmask=c_hmask), last, scan_steps, rw_parts)
        elif stages is None or 5 in stages:
            kb.begin()
            z = kb.sb("zz", [128, TOK])
            op('pool', lambda e: e.memset(z[:], 0.0), writes=['zz'])
            for fb in range(4):
                dma(lambda e, fb=fb: e.dma_start(out=YT[1][fb * 128:(fb + 1) * 128, :], in_=z[:]), reads=['zz'])
            kb.end()
        stage_on(6)
        chunks6 = CHUNKS[1:] if last else CHUNKS
        kb.begin()
        wbc = [kb.sb("wbc%d" % i, [128, 16, 128]) for i in range(2)]; wo = kb.sb("wo", [128, 8, D])
        yT = [kb.sb("yT%d" % i, [128, 4, 512]) for i in range(4)]
        gT = [kb.sb("gT%d" % i, [128, 512]) for i in range(2)]
        tmp = kb.sb("tmp6", [128, 512]); acc = kb.sb("acc", [128, 8, 512])
        xt = kb.sb("xt6", [128, D]); z = kb.sb("z6", [128, D]); z2 = kb.sb("z62", [128, D]); st = kb.sb("st6", [128, 4])
        pb = [kb.ps("pb%d" % i, [128, 512]) for i in range(2)]
        pmx = [kb.ps("pmx%d" % i, [128, 512]) for i in range(2)]
        dma(lambda e: e.dma_start(out=wo[:], in_=w_out[l].rearrange("(k p) n -> p k n", p=128)), writes=['wo'])

        def layer_norm(src, srck, dst, dstk, g, b, eps, n_=D):
            op('dve', lambda e: e.reduce_sum(st[:, 0:1], src[:], axis=AX.X), reads=[srck], writes=['st6'])
            op('dve', lambda e: e.tensor_scalar(st[:, 1:2], st[:, 0:1], -1.0 / n_, None, op0=ALU.mult), reads=['st6'], writes=['st6'])
            op('dve', lambda e: e.tensor_scalar(src[:], src[:], st[:, 1:2], None, op0=ALU.add), reads=['st6', srck], writes=[srck])
            op('dve', lambda e: e.scalar_tensor_tensor(out=dst[:], in0=src[:], scalar=1.0, in1=src[:], op0=ALU.mult, op1=ALU.mult, accum_out=st[:, 2:3]),
               reads=[srck], writes=[dstk, 'st6'])
            op('dve', lambda e: e.tensor_scalar(st[:, 3:4], st[:, 2:3], 1.0 / n_, eps, op0=ALU.mult, op1=ALU.add), reads=['st6'], writes=['st6'])
            op('act', lambda e: e.activation(st[:, 3:4], st[:, 3:4], AF.Sqrt), reads=['st6'], writes=['st6'])
            op('dve', lambda e: e.reciprocal(st[:, 3:4], st[:, 3:4]), reads=['st6'], writes=['st6'])
            op('dve', lambda e: e.scalar_tensor_tensor(out=dst[:], in0=src[:], scalar=st[:, 3:4], in1=g[:], op0=ALU.mult, op1=ALU.mult), reads=[srck, 'st6'], writes=[dstk])
            op('pool', lambda e: e.tensor_tensor(out=dst[:], in0=dst[:], in1=b[:], op=ALU.add), reads=[dstk], writes=[dstk])

        gc = 0
        for (n0, n) in chunks6:
            which = 1 if n0 == 0 else 0
            for i in range(4):
                dma(lambda e, i=i, n0=n0, n=n: e.dma_start(out=yT[i][:, :, 0:n], in_=YT[i][:, n0:n0 + n].rearrange("(k p) n -> p k n", p=128)), writes=['yT%d' % i])
            for c in range(8):
                WB = wbc[c % 2]; wbk = 'wbc%d' % (c % 2)
                dma(lambda e, WB=WB, c=c: e.dma_start(out=WB[:], in_=w_branch[l, :, :, c * 128:(c + 1) * 128].rearrange("i (k p) n -> p (i k) n", p=128)), writes=[wbk])
                for i in range(4):
                    G_ = gT[gc % 2]; gk_ = 'gT%d' % (gc % 2); PB = pb[gc % 2]; pbk = 'pb%d' % (gc % 2); gc += 1
                    r0 = OFF_GATE + i * D + c * 128
                    dma(lambda e, G_=G_, r0=r0, n0=n0, n=n: e.dma_start(out=G_[:, 0:n], in_=PT[r0:r0 + 128, n0:n0 + n]), writes=[gk_])
                    op('act', lambda e, G_=G_, n=n: e.activation(G_[:, 0:n], G_[:, 0:n], AF.Sigmoid), reads=[gk_], writes=[gk_])
                    for k in range(4):
                        op('pe', lambda e, PB=PB, WB=WB, i=i, k=k, n=n: e.matmul(PB[:, 0:n], WB[:, i * 4 + k, :], yT[i][:, k, 0:n], start=(k == 0), stop=(k == 3)),
                           reads=[wbk, 'yT%d' % i], writes=[pbk])
                    if i == 0:
                        op('dve', lambda e, PB=PB, G_=G_, c=c, n=n: e.tensor_tensor(out=acc[:, c, 0:n], in0=PB[:, 0:n], in1=G_[:, 0:n], op=ALU.mult), reads=[pbk, gk_], writes=['acc'])
                    else:
                        op('dve', lambda e, PB=PB, G_=G_, n=n: e.tensor_tensor(out=tmp[:, 0:n], in0=PB[:, 0:n], in1=G_[:, 0:n], op=ALU.mult), reads=[pbk, gk_], writes=['tmp6'])
                        op('pool', lambda e, c=c, n=n: e.tensor_tensor(out=acc[:, c, 0:n], in0=acc[:, c, 0:n], in1=tmp[:, 0:n], op=ALU.add), reads=['tmp6', 'acc'], writes=['acc'])
            for sub in range(n // 128):
                t = n0 // 128 + sub
                dma(lambda e, t=t: e.dma_start(out=xt[:], in_=XS[t * 128:(t + 1) * 128, :]), writes=['xt6'])
                for hf in range(2):
                    PM = pmx[hf]; pmk = 'pmx%d' % hf
                    for c in range(8):
                        op('pe', lambda e, PM=PM, c=c, sub=sub, hf=hf: e.matmul(PM[:], acc[:, c, sub * 128:(sub + 1) * 128], wo[:, c, hf * 512:(hf + 1) * 512], start=(c == 0), stop=(c == 7)),
                           reads=['acc', 'wo'], writes=[pmk])
                    op('dve', lambda e, PM=PM, hf=hf, which=which: e.tensor_tensor(out=z2[:, hf * 512:(hf + 1) * 512], in0=PM[:], in1=bc["gt1"][which][:, hf * 512:(hf + 1) * 512], op=ALU.mult),
                       reads=[pmk], writes=['z62'])
                op('dve', lambda e: e.scalar_tensor_tensor(out=z[:], in0=xt[:], scalar=ALPHA, in1=z2[:], op0=ALU.mult, op1=ALU.add), reads=['xt6', 'z62'], writes=['z6'])
                layer_norm(z, 'z6', z2, 'z62', lnp["g1"], lnp["b1"], 1e-5)
                dma(lambda e, t=t: e.dma_start(out=XMID[t * 128:(t + 1) * 128, :], in_=z2[:]), reads=['z62'])
        kb.end()
        if stop_after == 's6':
            break
        stage_on(7)
        kb.begin()
        tiles7 = list(range(2, NT)) if last else list(range(NT))
        if peer_tiles is not None:
            tiles7 = list(peer_tiles)
        wq = kb.sb("wq7", [128, 8, D]); keysT = kb.sb("keysT", [128, 8, 256]); kraw = kb.sb("kraw", [128, 16, 64])
        xm = kb.sb("xm7", [128, D]); hh = kb.sb("hh7", [128, D]); hT = kb.sb("hT7", [128, 8, 128]); qT = kb.sb("qT7", [128, 128])
        sc = kb.sb("sc7", [128, 16, 128]); wk = kb.sb("wk7", [128, 256])
        v16 = kb.sb("v16", [128, 16, 16]); i16 = kb.sb("i16", [128, 16, 16], U32); i16f = kb.sb("i16f", [128, 16, 16])
        cand = kb.sb("cand", [128, 8, 256]); best = kb.sb("best", [128, 8, 16]); pos = kb.sb("pos", [128, 8, 16], U32)
        pa = kb.sb("pa", [128, 8, 16], U32); pbi = kb.sb("pbi", [128, 8, 16], U32); paf = kb.sb("paf", [128, 8, 16]); pbf = kb.sb("pbf", [128, 8, 16])
        eq = kb.sb("eq7", [128, 8, 16, 16]); id1 = kb.sb("id1", [128, 8, 16]); id2 = kb.sb("id2", [128, 8, 16])
        eidf = kb.sb("eidf", [128, 128]); eid = kb.sb("eid", [128, 128], I32)
        wg = kb.sb("wg7", [128, 8, 16]); ssum = kb.sb("ssum7", [128, 8]); iota16 = kb.sb("iota16", [128, 16])
        apre = kb.sb("apre", [128, 128]); coef = kb.sb("coef", [128, 128])
        NB = 6
        rows = [kb.sb("row%d" % i, [128, D]) for i in range(NB)]
        junk = kb.sb("junk7", [128, D]); facc = kb.sb("facc", [128, D]); st = kb.sb("st6", [128, 4]); z = kb.sb("z7", [128, D])
        ptx = [kb.ps("p7t%d" % i, [128, 128]) for i in range(2)]
        pq7 = kb.ps("pq7", [128, 128]); psc = kb.ps("psc7", [128, 16, 128])
        dma(lambda e: e.dma_start(out=wq[:], in_=peer_wq[l].rearrange("(k p) n -> p k n", p=128)), writes=['wq7'])
        dma(lambda e: e.dma_start(out=kraw[:], in_=peer_keys[l].rearrange("h s k d -> k (h s) d")), writes=['kraw'])
        dma(lambda e: e.dma_start(out=iota16[:], in_=c_iota16), writes=['iota16'])
        op('pool', lambda e: e.memset(keysT[:], 0.0), writes=['keysT'])
        for h in range(8):
            op('pe', lambda e, h=h: e.transpose(ptx[h % 2][:], kraw[:, 2 * h:2 * h + 2, :].rearrange("p a d -> p (a d)"), ident[:]), reads=['kraw'], writes=['p7t%d' % (h % 2)])
            for s_ in range(2):
                op('act', lambda e, h=h, s_=s_: e.copy(keysT[64 * s_:64 * s_ + 64, h, 128 * s_:128 * s_ + 128], ptx[h % 2][64 * s_:64 * s_ + 64, :]), reads=['p7t%d' % (h % 2)], writes=['keysT'])
        rc = 0
        for t in tiles7:
            sub7(2)
            which = 1 if t < 2 else 0
            dma(lambda e, t=t: e.dma_start(out=xm[:], in_=XMID[t * 128:(t + 1) * 128, :]), writes=['xm7'])
            op('dve', lambda e, which=which: e.tensor_tensor(out=hh[:], in0=xm[:], in1=bc["sc2"][which][:], op=ALU.mult), reads=['xm7'], writes=['hh7'])
            op('dve', lambda e, which=which: e.tensor_tensor(out=hh[:], in0=hh[:], in1=bc["sh2"][which][:], op=ALU.add), reads=['hh7'], writes=['hh7'])
            for k in range(8):
                op('pe', lambda e, k=k: e.transpose(ptx[k % 2][:], hh[:, k * 128:(k + 1) * 128], ident[:]), reads=['hh7'], writes=['p7t%d' % (k % 2)])
                op('act', lambda e, k=k: e.copy(hT[:, k, :], ptx[k % 2][:]), reads=['p7t%d' % (k % 2)], writes=['hT7'])
            for h in range(8):
                sub7(2.3)
                for k in range(8):
                    op('pe', lambda e, h=h, k=k: e.matmul(pq7[:], wq[:, k, h * 128:(h + 1) * 128], hT[:, k, :], start=(k == 0), stop=(k == 7)), reads=['wq7', 'hT7'], writes=['pq7'])
                op('act', lambda e: e.copy(qT[:], pq7[:]), reads=['pq7'], writes=['qT7'])
                sub7(2.6)
                op('pe', lambda e, h=h: e.matmul(psc[:, 2 * h:2 * h + 2, :].rearrange("p a k -> p (a k)"), qT[:, :], keysT[:, h, :], start=True, stop=True),
                   reads=['qT7', 'keysT'], writes=['psc7'])
            sub7(3)
            for q4 in range(4):
                op('act', lambda e, q4=q4: e.copy(sc[:, 4 * q4:4 * q4 + 4, :], psc[:, 4 * q4:4 * q4 + 4, :]), reads=['psc7'], writes=['sc7'])

            def top16(src, vdst, idst):
                N = src.shape[-1]
                op('dve', lambda e: e.max(out=vdst[:, 0:8], in_=src), reads=['sc7', 'cand'], writes=['v16'])
                op('dve', lambda e: e.match_replace(out=wk[:, 0:N], in_to_replace=vdst[:, 0:8], in_values=src, imm_value=-1e30), reads=['v16', 'sc7', 'cand'], writes=['wk7'])
                op('dve', lambda e: e.max(out=vdst[:, 8:16], in_=wk[:, 0:N]), reads=['wk7'], writes=['v16'])
                op('dve', lambda e: e.max_index(out=idst[:, 0:8], in_max=vdst[:, 0:8], in_values=src), reads=['v16', 'sc7', 'cand'], writes=['i16'])
                op('dve', lambda e: e.max_index(out=idst[:, 8:16], in_max=vdst[:, 8:16], in_values=src), reads=['v16', 'sc7', 'cand'], writes=['i16'])

            sub7(4)
            for j in range(16):
                top16(sc[:, j, :], v16[:, j, :], i16[:, j, :])
            v4 = v16[:].rearrange("p (h s) a -> p h s a", s=2)
            op('dve', lambda e: e.tensor_tensor(out=cand[:].rearrange("p h (a b) -> p h a b", b=16), in0=v4[:, :, 0, :].unsqueeze(3).to_broadcast([128, 8, 16, 16]),
                                                 in1=v4[:, :, 1, :].unsqueeze(2).to_broadcast([128, 8, 16, 16]), op=ALU.add), reads=['v16'], writes=['cand'])
            for h in range(8):
                top16(cand[:, h, :], best[:, h, :], pos[:, h, :])
            sub7(5)
            op('dve', lambda e: e.tensor_tensor(out=wg[:], in0=best[:], in1=best[:, :, 0:1].to_broadcast([128, 8, 16]), op=ALU.subtract), reads=['v16'], writes=['wg7'])
            op('act', lambda e: e.activation(wg[:], wg[:], AF.Exp), reads=['wg7'], writes=['wg7'])
            op('dve', lambda e: e.reduce_sum(ssum[:], wg[:], axis=AX.X), reads=['wg7'], writes=['ssum7'])
            op('dve', lambda e: e.reciprocal(ssum[:], ssum[:]), reads=['ssum7'], writes=['ssum7'])
            op('dve', lambda e: e.tensor_tensor(out=wg[:], in0=wg[:], in1=ssum[:].unsqueeze(2).to_broadcast([128, 8, 16]), op=ALU.mult), reads=['wg7', 'ssum7'], writes=['wg7'])
            op('dve', lambda e: e.tensor_single_scalar(pa[:], pos[:], 4, op=ALU.logical_shift_right), reads=['i16'], writes=['pa'])
            op('dve', lambda e: e.tensor_single_scalar(pbi[:], pos[:], 15, op=ALU.bitwise_and), reads=['i16'], writes=['pbi'])
            op('dve', lambda e: e.tensor_copy(paf[:], pa[:]), reads=['pa'], writes=['paf'])
            op('dve', lambda e: e.tensor_copy(pbf[:], pbi[:]), reads=['pbi'], writes=['pbf'])
            op('dve', lambda e: e.tensor_copy(i16f[:], i16[:]), reads=['i16'], writes=['i16f'])
            i4 = i16f[:].rearrange("p (h s) a -> p h s a", s=2)
            for (pf, s, dst, dk) in ((paf, 0, id1, 'id1'), (pbf, 1, id2, 'id2')):
                op('dve', lambda e, pf=pf: e.tensor_tensor(out=eq[:], in0=pf[:].unsqueeze(3).to_broadcast([128, 8, 16, 16]),
                                                          in1=iota16[:].unsqueeze(1).unsqueeze(1).to_broadcast([128, 8, 16, 16]), op=ALU.is_equal),
                   reads=['paf', 'pbf', 'iota16'], writes=['eq7'])
                op('dve', lambda e, s=s: e.tensor_tensor(out=eq[:], in0=eq[:], in1=i4[:, :, s, :].unsqueeze(2).to_broadcast([128, 8, 16, 16]), op=ALU.mult),
                   reads=['eq7', 'i16f'], writes=['eq7'])
                op('dve', lambda e, dst=dst: e.reduce_sum(dst[:], eq[:], axis=AX.X), reads=['eq7'], writes=[dk])
            op('dve', lambda e: e.scalar_tensor_tensor(out=eidf[:], in0=id1[:].rearrange("p h k -> p (h k)"), scalar=128.0, in1=id2[:].rearrange("p h k -> p (h k)"), op0=ALU.mult, op1=ALU.add),
               reads=['id1', 'id2'], writes=['eidf'])
            if l > 0:
                op('dve', lambda e: e.tensor_scalar_add(eidf[:], eidf[:], float(l * 16384)), reads=['eidf'], writes=['eidf'])
            op('dve', lambda e: e.tensor_scalar(eidf[:], eidf[:], 0.0, 32767.0, op0=ALU.max, op1=ALU.min), reads=['eidf'], writes=['eidf'])
            op('dve', lambda e: e.tensor_copy(eid[:], eidf[:]), reads=['eidf'], writes=['eid'])
            sub7(6)
            for e_ in range(peer_slots):
                R = rows[rc % NB]; rk_ = 'row%d' % (rc % NB); rc += 1
                dma(lambda e, R=R, e_=e_: e.indirect_dma_start(out=R[:], out_offset=None, in_=peer_u.rearrange("l e d -> (l e) d"), in_offset=bass.IndirectOffsetOnAxis(ap=eid[:, e_:e_ + 1], axis=0)),
                    reads=['eid'], writes=[rk_], q='pool')
                op('dve', lambda e, R=R, e_=e_: e.scalar_tensor_tensor(out=junk[:], in0=R[:], scalar=1.0, in1=hh[:], op0=ALU.mult, op1=ALU.mult, accum_out=apre[:, e_:e_ + 1]),
                   reads=[rk_, 'hh7'], writes=['junk7', 'apre'])
            sub7(7)
            op('act', lambda e: e.activation(coef[:], apre[:], AF.Gelu), reads=['apre'], writes=['coef'])
            op('dve', lambda e: e.tensor_tensor(out=coef[:], in0=coef[:], in1=wg[:].rearrange("p h k -> p (h k)"), op=ALU.mult), reads=['coef', 'wg7'], writes=['coef'])
            for e_ in range(peer_slots):
                R = rows[rc % NB]; rk_ = 'row%d' % (rc % NB); rc += 1
                dma(lambda e, R=R, e_=e_: e.indirect_dma_start(out=R[:], out_offset=None, in_=peer_v.rearrange("l e d -> (l e) d"), in_offset=bass.IndirectOffsetOnAxis(ap=eid[:, e_:e_ + 1], axis=0)),
                    reads=['eid'], writes=[rk_], q='pool')
                if e_ == 0:
                    op('dve', lambda e, R=R: e.tensor_scalar(facc[:], R[:], coef[:, 0:1], None, op0=ALU.mult), reads=[rk_, 'coef'], writes=['facc'])
                else:
                    op('dve', lambda e, R=R, e_=e_: e.scalar_tensor_tensor(out=facc[:], in0=R[:], scalar=coef[:, e_:e_ + 1], in1=facc[:], op0=ALU.mult, op1=ALU.add),
                       reads=[rk_, 'coef', 'facc'], writes=['facc'])
            op('dve', lambda e, which=which: e.tensor_tensor(out=facc[:], in0=facc[:], in1=bc["gt2"][which][:], op=ALU.mult), reads=['facc'], writes=['facc'])
            op('dve', lambda e: e.scalar_tensor_tensor(out=z[:], in0=xm[:], scalar=ALPHA, in1=facc[:], op0=ALU.mult, op1=ALU.add), reads=['xm7', 'facc'], writes=['z7'])
            layer_norm(z, 'z7', facc, 'facc', lnp["g2"], lnp["b2"], 1e-5)
            if last:
                dma(lambda e, t=t: e.dma_start(out=yout[(t - 2) * 128:(t - 1) * 128, :], in_=facc[:]), reads=['facc'])
            else:
                dma(lambda e, t=t: e.dma_start(out=XN[t * 128:(t + 1) * 128, :], in_=facc[:]), reads=['facc'])
        stage_on(7)
        kb.end()
        kb.end()
    p.barrier()
    return kb


def rwkv_stage(kb, l, PT, Yd, rw, cst, last, scan_steps=None, parts=None):
    nc, p = kb.nc, kb.p
    on = [True]

    def op(*a, **k):
        if on[0]:
            p.op(*a, **k)

    def dma(*a, **k):
        if on[0]:
            p.dma(*a, **k)

    def part(x):
        on[0] = parts is None or x in parts
    ident, ones, bones, c_hmask = cst['ident'], cst['ones'], cst['bones'], cst['c_hmask']
    sc = kb.scratch
    RM = sc("RM%d" % l, [1920, TOK])
    KKT = sc("KKT%d" % l, [512, TOK]); WT = [sc("WT%d_%d" % (l, d), [512, TOK]) for d in range(2)]
    TR = sc("TR%d" % l, [TOK, 512]); TV = sc("TV%d" % l, [TOK, 512]); TG = sc("TG%d" % l, [TOK, 512])
    TKD = [sc("TKD%d_%d" % (l, d), [TOK, 512]) for d in range(2)]
    TKA = [sc("TKA%d_%d" % (l, d), [TOK, 512]) for d in range(2)]
    YS = [sc("YS%d_%d" % (l, d), [8, TOK, 64]) for d in range(2)]
    part('A')
    kb.begin()
    XP = [kb.sb("XP%d" % i, [128, TOK + 8]) for i in range(2)]
    MO = [kb.sb("MO%d" % i, [128, TOK]) for i in range(2)]
    mu = kb.sb("mu", [128, 15, 2]); c0 = kb.sb("c0", [128, 15])
    for j in range(2):
        dma(lambda e, j=j: e.dma_start(out=mu[:, :, j], in_=rw["rwkv_mu"][l, j].rearrange("(c p) -> p c", p=128), allow_slow_non_contiguous=True), writes=['mu'])
    op('dve', lambda e: e.tensor_tensor(out=c0[:], in0=mu[:, :, 0], in1=mu[:, :, 1], op=ALU.add), reads=['mu'], writes=['c0'])
    op('dve', lambda e: e.tensor_scalar(c0[:], c0[:], -1.0, 1.0, op0=ALU.mult, op1=ALU.add), reads=['c0'], writes=['c0'])
    for i in range(2):
        op('pool', lambda e, i=i: e.memset(XP[i][:], 0.0), writes=['XP%d' % i])
    for c in range(15):
        X = XP[c % 2]; xk = 'XP%d' % (c % 2); M = MO[c % 2]; mk = 'MO%d' % (c % 2)
        r0 = OFF_RWKV + c * 128
        dma(lambda e, X=X, r0=r0: e.dma_start(out=X[:, 1:257], in_=PT[r0:r0 + 128, 0:256]), writes=[xk])
        dma(lambda e, X=X, r0=r0: e.dma_start(out=X[:, 259:259 + SEQ], in_=PT[r0:r0 + 128, 256:TOK]), writes=[xk])
        for (o0, n, x0) in ((0, 256, 1), (256, SEQ, 259)):
            op('dve', lambda e, X=X, M=M, o0=o0, n=n, x0=x0, c=c: e.tensor_scalar(M[:, o0:o0 + n], X[:, x0:x0 + n], c0[:, c:c + 1], None, op0=ALU.mult), reads=[xk, 'c0'], writes=[mk])
            op('dve', lambda e, X=X, M=M, o0=o0, n=n, x0=x0, c=c: e.scalar_tensor_tensor(out=M[:, o0:o0 + n], in0=X[:, x0 - 1:x0 - 1 + n], scalar=mu[:, c, 0:1], in1=M[:, o0:o0 + n], op0=ALU.mult, op1=ALU.add),
               reads=[xk, 'mu', mk], writes=[mk])
            op('dve', lambda e, X=X, M=M, o0=o0, n=n, x0=x0, c=c: e.scalar_tensor_tensor(out=M[:, o0:o0 + n], in0=X[:, x0 + 1:x0 + 1 + n], scalar=mu[:, c, 1:2], in1=M[:, o0:o0 + n], op0=ALU.mult, op1=ALU.add),
               reads=[xk, 'mu', mk], writes=[mk])
        dma(lambda e, M=M, c=c: e.dma_start(out=RM[c * 128:(c + 1) * 128, :], in_=M[:]), reads=[mk])
    kb.end()
    part('B')
    kb.begin()
    pv_ = {}
    def col(name, src, ncol):
        t = kb.sb(name, [128, ncol])
        dma(lambda e: e.dma_start(out=t[:], in_=src.rearrange("(c p) -> p c", p=128), allow_slow_non_contiguous=True), writes=[name])
        return t
    kkc = col("kkc", rw["rwkv_k_k"][l], 4); kac = col("kac", rw["rwkv_k_a"][l], 4); rkc = col("rkc", rw["rwkv_r_k"][l], 4)
    w0c = [col("w0c%d" % d, rw["rwkv_w0"][l, d], 4) for d in range(2)]
    a0c = [col("a0c%d" % d, rw["rwkv_a0"][l, d], 4) for d in range(2)]
    omka = kb.sb("omka", [128, 4])
    op('dve', lambda e: e.tensor_scalar(omka[:], kac[:], -1.0, 1.0, op0=ALU.mult, op1=ALU.add), reads=['kac'], writes=['omka'])
    w2 = kb.sb("w2", [128, 512]); a2 = kb.sb("a2", [128, 512]); g2 = kb.sb("g2", [128, 512])
    dma(lambda e: e.dma_start(out=w2[:], in_=rw["rwkv_w2"][l].rearrange("d k n -> (d k) n")), writes=['w2'])
    dma(lambda e: e.dma_start(out=a2[:], in_=rw["rwkv_a2"][l].rearrange("d k n -> (d k) n")), writes=['a2'])
    dma(lambda e: e.dma_start(out=g2[:], in_=rw["rwkv_g2"][l]), writes=['g2'])
    Rt = kb.sb("Rt", [128, 4, 512]); Kt = kb.sb("Kt", [128, 4, 512]); Vt = kb.sb("Vt", [128, 4, 512])
    wfb = kb.sb("wfb", [128, 512]); afb = kb.sb("afb", [128, 512]); gi = kb.sb("gi", [128, 512])
    kk = kb.sb("kkB", [128, 4, 512]); tB = kb.sb("tB", [128, 512]); sqB = kb.sb("sqB", [128, 512]); nB = kb.sb("nB", [128, 512])
    aT = kb.sb("aT", [128, 512]); fo = [kb.sb("fo%d" % i, [128, 512]) for i in range(3)]
    tm = {nm: kb.sb("tm_" + nm, [128, 4, 512]) for nm in ("kd0", "kd1", "ka0", "ka1", "g")}
    tok = [kb.sb("tokB%d" % i, [128, 512]) for i in range(2)]
    pB = [kb.ps("pB%d" % i, [128, 512]) for i in range(3)]
    pT = [kb.ps("pTB%d" % i, [128, 512]) for i in range(2)]
    fc = 0; tc_ = 0
    NEG = -math.exp(-0.5)
    for (n0, n) in CHUNKS:
        for (T, nm, r0) in ((Rt, 'Rt', 0), (Kt, 'Kt', 512), (Vt, 'Vt', 1024)):
            dma(lambda e, T=T, r0=r0, n0=n0, n=n: e.dma_start(out=T[:, :, 0:n], in_=RM[r0:r0 + 512, n0:n0 + n].rearrange("(c p) n -> p c n", p=128)), writes=[nm])
        dma(lambda e, n0=n0, n=n: e.dma_start(out=wfb[:, 0:n], in_=RM[1536:1664, n0:n0 + n]), writes=['wfb'])
        dma(lambda e, n0=n0, n=n: e.dma_start(out=afb[:, 0:n], in_=RM[1664:1792, n0:n0 + n]), writes=['afb'])
        dma(lambda e, n0=n0, n=n: e.dma_start(out=gi[:, 0:n], in_=RM[1792:1920, n0:n0 + n]), writes=['gi'])
        op('act', lambda e, n=n: e.activation(wfb[:, 0:n], wfb[:, 0:n], AF.Tanh), reads=['wfb'], writes=['wfb'])
        op('act', lambda e, n=n: e.activation(gi[:, 0:n], gi[:, 0:n], AF.Sigmoid), reads=['gi'], writes=['gi'])
        for c in range(4):
            op('dve', lambda e, c=c, n=n: e.tensor_scalar(tB[:, 0:n], Kt[:, c, 0:n], kkc[:, c:c + 1], None, op0=ALU.mult), reads=['Kt', 'kkc'], writes=['tB'])
            op('act', lambda e, n=n: e.activation(sqB[:, 0:n], tB[:, 0:n], AF.Square), reads=['tB'], writes=['sqB'])
            P_ = pB[fc % 3]; pk_ = 'pB%d' % (fc % 3); fc += 1
            op('pe', lambda e, P_=P_, n=n: e.matmul(P_[:, 0:n], bones[:], sqB[:, 0:n], start=True, stop=True), reads=['sqB'], writes=[pk_])
            op('act', lambda e, P_=P_, n=n: e.activation(nB[:, 0:n], P_[:, 0:n], AF.Sqrt), reads=[pk_], writes=['nB'])
            op('dve', lambda e, n=n: e.tensor_scalar(nB[:, 0:n], nB[:, 0:n], 1e-12, None, op0=ALU.max), reads=['nB'], writes=['nB'])
            op('dve', lambda e, n=n: e.reciprocal(nB[:, 0:n], nB[:, 0:n]), reads=['nB'], writes=['nB'])
            op('dve', lambda e, c=c, n=n: e.tensor_tensor(out=kk[:, c, 0:n], in0=tB[:, 0:n], in1=nB[:, 0:n], op=ALU.mult), reads=['tB', 'nB'], writes=['kkB'])
            for d in range(2):
                P_ = pB[fc % 3]; pk_ = 'pB%d' % (fc % 3); fc += 1
                F = fo[fc % 3]; fk = 'fo%d' % (fc % 3)
                op('pe', lambda e, P_=P_, c=c, d=d, n=n: e.matmul(P_[:, 0:n], w2[64 * d:64 * d + 64, c * 128:(c + 1) * 128], wfb[64 * d:64 * d + 64, 0:n], start=True, stop=True),
                   reads=['w2', 'wfb'], writes=[pk_])
                op('act', lambda e, P_=P_, F=F, c=c, d=d, n=n: e.activation(F[:, 0:n], P_[:, 0:n], AF.Sigmoid, bias=w0c[d][:, c:c + 1]), reads=[pk_], writes=[fk])
                op('act', lambda e, F=F, n=n: e.activation(F[:, 0:n], F[:, 0:n], AF.Exp, scale=NEG), reads=[fk], writes=[fk])
                dma(lambda e, F=F, c=c, d=d, n0=n0, n=n: e.dma_start(out=WT[d][c * 128:(c + 1) * 128, n0:n0 + n], in_=F[:, 0:n]), reads=[fk])
                P_ = pB[fc % 3]; pk_ = 'pB%d' % (fc % 3); fc += 1
                op('pe', lambda e, P_=P_, c=c, d=d, n=n: e.matmul(P_[:, 0:n], a2[64 * d:64 * d + 64, c * 128:(c + 1) * 128], afb[64 * d:64 * d + 64, 0:n], start=True, stop=True),
                   reads=['a2', 'afb'], writes=[pk_])
                op('act', lambda e, P_=P_, c=c, d=d, n=n: e.activation(aT[:, 0:n], P_[:, 0:n], AF.Sigmoid, bias=a0c[d][:, c:c + 1]), reads=[pk_], writes=['aT'])
                op('dve', lambda e, c=c, d=d, n=n: e.scalar_tensor_tensor(out=tm["ka%d" % d][:, c, 0:n], in0=kk[:, c, 0:n], scalar=-1.0, in1=aT[:, 0:n], op0=ALU.mult, op1=ALU.mult),
                   reads=['kkB', 'aT'], writes=['tm_ka%d' % d])
                op('dve', lambda e, c=c, n=n: e.tensor_scalar(aT[:, 0:n], aT[:, 0:n], kac[:, c:c + 1], omka[:, c:c + 1], op0=ALU.mult, op1=ALU.add), reads=['aT', 'kac', 'omka'], writes=['aT'])
                op('dve', lambda e, c=c, d=d, n=n: e.tensor_tensor(out=tm["kd%d" % d][:, c, 0:n], in0=Kt[:, c, 0:n], in1=aT[:, 0:n], op=ALU.mult), reads=['Kt', 'aT'], writes=['tm_kd%d' % d])
            dma(lambda e, c=c, n0=n0, n=n: e.dma_start(out=KKT[c * 128:(c + 1) * 128, n0:n0 + n], in_=kk[:, c, 0:n]), reads=['kkB'])
            P_ = pB[fc % 3]; pk_ = 'pB%d' % (fc % 3); fc += 1
            op('pe', lambda e, P_=P_, c=c, n=n: e.matmul(P_[:, 0:n], g2[:, c * 128:(c + 1) * 128], gi[:, 0:n], start=True, stop=True), reads=['g2', 'gi'], writes=[pk_])
            op('act', lambda e, P_=P_, c=c, n=n: e.copy(tm["g"][:, c, 0:n], P_[:, 0:n]), reads=[pk_], writes=['tm_g'])
        for (src, sk, dst) in ((Rt, 'Rt', TR), (Vt, 'Vt', TV), (tm["g"], 'tm_g', TG), (tm["kd0"], 'tm_kd0', TKD[0]), (tm["kd1"], 'tm_kd1', TKD[1]),
                               (tm["ka0"], 'tm_ka0', TKA[0]), (tm["ka1"], 'tm_ka1', TKA[1])):
            for sub in range(n // 128):
                P_ = pT[tc_ % 2]; pk_ = 'pTB%d' % (tc_ % 2); O = tok[tc_ % 2]; ok_ = 'tokB%d' % (tc_ % 2); tc_ += 1
                for c in range(4):
                    op('pe', lambda e, P_=P_, src=src, c=c, sub=sub: e.transpose(P_[:, c * 128:(c + 1) * 128], src[:, c, sub * 128:(sub + 1) * 128], ident[:]), reads=[sk], writes=[pk_])
                if tc_ % 2:
                    op('act', lambda e, P_=P_, O=O: e.copy(O[:], P_[:]), reads=[pk_], writes=[ok_])
                else:
                    op('dve', lambda e, P_=P_, O=O: e.tensor_copy(O[:], P_[:]), reads=[pk_], writes=[ok_])
                t0 = n0 + sub * 128
                dma(lambda e, O=O, dst=dst, t0=t0: e.dma_start(out=dst[t0:t0 + 128, :], in_=O[:]), reads=[ok_])
    kb.end()
    part('C')
    kb.begin()
    TS = 8
    S = kb.sb("S", [128, 512]); tS = kb.sb("tS", [128, 512]); hmask = kb.sb("hmask", [128, 512])
    A = [kb.sb("A%d" % i, [128, TS, 40]) for i in range(2)]
    sKK = [kb.sb("sKK%d" % i, [128, 8, TS + 1]) for i in range(2)]
    sR = [kb.sb("sR%d" % i, [128, 8, TS]) for i in range(2)]
    sW = [kb.sb("sW%d" % i, [128, 8, TS]) for i in range(2)]
    U = [kb.sb("U%d" % i, [128, TS, 64]) for i in range(2)]
    V2 = [kb.sb("V2%d" % i, [128, TS, 512]) for i in range(2)]
    Yb = [kb.sb("Yb%d" % i, [128, TS, 512]) for i in range(2)]
    ya = [kb.sb("ya%d" % i, [128, TS, 64]) for i in range(2)]
    P1 = [kb.ps("P1_%d" % i, [128, 512]) for i in range(2)]
    PU = [kb.ps("PU_%d" % i, [128, 512]) for i in range(2)]
    dma(lambda e: e.dma_start(out=hmask[:], in_=c_hmask), writes=['hmask'])
    op('pool', lambda e: e.memset(S[:], 0.0), writes=['S'])
    for i in range(2):
        for (T, nm) in ((A, 'A'), (U, 'U'), (sKK, 'sKK')):
            op('pool', lambda e, T=T, i=i: e.memset(T[i][:], 0.0), writes=['%s%d' % (nm, i)])
        op('pool', lambda e, i=i: e.memset(V2[i][:], 0.0), writes=['V2v%d' % i, 'V2s%d' % i])
    nsteps = scan_steps or TOK
    def tok_of(d, s):
        if d == 0:
            return s
        return 255 - s if s < 256 else 4607 - s
    def lo_of(ch):
        return [min(tok_of(d, ch * TS), tok_of(d, ch * TS + TS - 1)) for d in range(2)]
    for ch in range(nsteps // TS):
        b = ch % 2
        s0 = ch * TS
        lo = lo_of(ch)
        for d in range(2):
            pr = slice(64 * d, 64 * d + 64)
            rv = slice(64 * d, 64 * d + 8)
            rs = slice(64 * d + 32, 64 * d + 40)
            l0 = lo[d]
            if d == 0:
                nload = TS + 1 if l0 + TS < TOK else TS
                dma(lambda e, pr=pr, l0=l0, nload=nload, b=b: e.dma_start(out=sKK[b][pr, :, 0:nload], in_=KKT[:, l0:l0 + nload].rearrange("(h j) t -> j h t", j=64)), writes=['sKK%d' % b])
            else:
                if l0 >= 1 and l0 != 256:
                    dma(lambda e, pr=pr, l0=l0, b=b: e.dma_start(out=sKK[b][pr, :, 0:TS + 1], in_=KKT[:, l0 - 1:l0 + TS].rearrange("(h j) t -> j h t", j=64)), writes=['sKK%d' % b])
                else:
                    dma(lambda e, pr=pr, l0=l0, b=b: e.dma_start(out=sKK[b][pr, :, 1:TS + 1], in_=KKT[:, l0:l0 + TS].rearrange("(h j) t -> j h t", j=64)), writes=['sKK%d' % b])
                    if l0 == 0:
                        dma(lambda e, pr=pr, b=b: e.dma_start(out=sKK[b][pr, :, 0:1], in_=KKT[:, TOK - 1:TOK].rearrange("(h j) t -> j h t", j=64), allow_slow_non_contiguous=True), writes=['sKK%d' % b])
            dma(lambda e, pr=pr, l0=l0, b=b: e.dma_start(out=sR[b][pr, :, :], in_=RM[0:512, l0:l0 + TS].rearrange("(h j) t -> j h t", j=64)), writes=['sR%d' % b])
            dma(lambda e, pr=pr, l0=l0, b=b, d=d: e.dma_start(out=sW[b][pr, :, :], in_=WT[d][:, l0:l0 + TS].rearrange("(h j) t -> j h t", j=64)), writes=['sW%d' % b], q='pool')
            sh = 1 if d == 0 else 0
            op('act', lambda e, pr=pr, b=b, sh=sh: e.copy(A[b][pr, :, 32:40].rearrange("p t h -> p h t"), sKK[b][pr, :, sh:sh + TS]), reads=['sKK%d' % b], writes=['A%d' % b])
            op('act', lambda e, pr=pr, b=b: e.copy(A[b][pr, :, 0:8].rearrange("p t h -> p h t"), sR[b][pr, :, :]), reads=['sR%d' % b], writes=['A%d' % b])
            dma(lambda e, rs=rs, l0=l0, b=b, d=d: e.dma_start(out=U[b][rs, :, :], in_=TKA[d][l0:l0 + TS, :].rearrange("t (h j) -> h t j", j=64)), writes=['U%d' % b])
            dma(lambda e, rv=rv, l0=l0, b=b, d=d: e.dma_start(out=U[b][rv, :, :], in_=TKD[d][l0:l0 + TS, :].rearrange("t (h j) -> h t j", j=64)), writes=['U%d' % b], q='pool')
            dma(lambda e, rv=rv, l0=l0, b=b: e.dma_start(out=V2[b][rv, :, :], in_=TV[l0:l0 + TS, :].partition_broadcast(8)), writes=['V2v%d' % b])
            op('pool', lambda e, b=b, rv=rv: e.tensor_tensor(out=V2[b][rv, :, :], in0=V2[b][rv, :, :], in1=hmask[rv, :].unsqueeze(1).to_broadcast([8, TS, 512]), op=ALU.mult),
               reads=['V2v%d' % b, 'hmask'], writes=['V2v%d' % b])
        for si in range(TS):
            s = s0 + si
            pos = [tok_of(d, s) - lo[d] for d in range(2)]
            q = s % 2
            for d in range(2):
                pr = slice(64 * d, 64 * d + 64)
                rk = slice(64 * d, 64 * d + 40)
                op('pe', lambda e, q=q, b=b, rk=rk, pr=pr, pd=pos[d]: e.matmul(PU[q][pr, :], U[b][rk, pd, :], V2[b][rk, pd, :], start=True, stop=True),
                   reads=['U%d' % b, 'V2v%d' % b, 'V2s%d' % b], writes=['PU_%d' % q])
            for d in range(2):
                pr = slice(64 * d, 64 * d + 64)
                op('pool', lambda e, pr=pr, b=b, pd=pos[d]: e.tensor_tensor(out=tS[pr, :].rearrange("p (h i) -> p h i", i=64), in0=S[pr, :].rearrange("p (h i) -> p h i", i=64),
                                                                          in1=sW[b][pr, :, pd].unsqueeze(2).to_broadcast([64, 8, 64]), op=ALU.mult),
                   reads=['S', 'sW%d' % b], writes=['tS'])
            op('dve', lambda e, q=q: e.tensor_tensor(out=S[:], in0=tS[:], in1=PU[q][:], op=ALU.add), reads=['tS', 'PU_%d' % q], writes=['S'])
            for d in range(2):
                pr = slice(64 * d, 64 * d + 64)
                op('pe', lambda e, q=q, b=b, pr=pr, pd=pos[d], d=d: e.matmul(P1[q][64 * d:64 * d + 40, :], A[b][pr, pd, :], S[pr, :], start=True, stop=True),
                   reads=['A%d' % b, 'S'], writes=['P1_%d' % q])
            if s + 1 < nsteps:
                chn = (s + 1) // TS
                bn = chn % 2
                lon = lo_of(chn)
                for d in range(2):
                    rs = slice(64 * d + 32, 64 * d + 40)
                    pn = tok_of(d, s + 1) - lon[d]
                    op('dve', lambda e, q=q, rs=rs, bn=bn, pn=pn: e.tensor_tensor(out=V2[bn][rs, pn, :], in0=P1[q][rs, :], in1=hmask[rs, :], op=ALU.mult),
                       reads=['P1_%d' % q, 'hmask'], writes=['V2s%d' % bn])
            for d in range(2):
                yr = slice(64 * d, 64 * d + 8)
                op('act', lambda e, q=q, b=b, yr=yr, pd=pos[d]: e.copy(Yb[b][yr, pd, :], P1[q][yr, :]), reads=['P1_%d' % q], writes=['Yb%d' % b])
        for d in range(2):
            yr = slice(64 * d, 64 * d + 8)
            op('pool', lambda e, b=b, yr=yr: e.tensor_tensor(out=Yb[b][yr, :, :], in0=Yb[b][yr, :, :], in1=hmask[yr, :].unsqueeze(1).to_broadcast([8, TS, 512]), op=ALU.mult),
               reads=['Yb%d' % b, 'hmask'], writes=['Yb%d' % b])
            op('pool', lambda e, b=b, yr=yr: e.tensor_tensor(out=Yb[b][yr, :, 0:256], in0=Yb[b][yr, :, 0:256], in1=Yb[b][yr, :, 256:512], op=ALU.add), reads=['Yb%d' % b], writes=['Yb%d' % b])
            op('pool', lambda e, b=b, yr=yr: e.tensor_tensor(out=Yb[b][yr, :, 0:128], in0=Yb[b][yr, :, 0:128], in1=Yb[b][yr, :, 128:256], op=ALU.add), reads=['Yb%d' % b], writes=['Yb%d' % b])
            op('pool', lambda e, b=b, yr=yr: e.tensor_tensor(out=ya[b][yr, :, :], in0=Yb[b][yr, :, 0:64], in1=Yb[b][yr, :, 64:128], op=ALU.add), reads=['Yb%d' % b], writes=['ya%d' % b])
            dma(lambda e, b=b, yr=yr, d=d, l0=lo[d]: e.dma_start(out=YS[d][:, l0:l0 + TS, :], in_=ya[b][yr, :, :]), reads=['ya%d' % b])
    kb.end()
    part('D')
    kb.begin()
    lng = kb.sb("lng", [128, 512]); lnb = kb.sb("lnb", [128, 512]); rkb = kb.sb("rkb", [128, 512])
    dma(lambda e: e.dma_start(out=lng[:], in_=rw["rwkv_ln_g"][l].partition_broadcast(128)), writes=['lng'])
    dma(lambda e: e.dma_start(out=lnb[:], in_=rw["rwkv_ln_b"][l].partition_broadcast(128)), writes=['lnb'])
    dma(lambda e: e.dma_start(out=rkb[:], in_=rw["rwkv_r_k"][l].partition_broadcast(128)), writes=['rkb'])
    o0 = kb.sb("o0", [128, 8, 64]); o1 = kb.sb("o1", [128, 8, 64]); sq = kb.sb("osq", [128, 8, 64])
    mean = kb.sb("omean", [128, 8]); var = kb.sb("ovar", [128, 8])
    tr = kb.sb("otr", [128, 512]); tv = kb.sb("otv", [128, 512]); tg = kb.sb("otg", [128, 512]); k0 = kb.sb("ok0", [128, 512]); k1 = kb.sb("ok1", [128, 512])
    bon = kb.sb("obon", [128, 8])
    oT = [kb.sb("oT%d" % i, [128, 128]) for i in range(2)]
    pT = [kb.ps("pTD%d" % i, [128, 128]) for i in range(2)]
    tiles = list(range(2, NT)) if last else list(range(NT))
    if scan_steps:
        tiles = [0, 1]
    ct = 0
    v3 = lambda t: t[:].rearrange("p (h i) -> p h i", i=64)
    for t in tiles:
        t0 = t * 128
        dma(lambda e, t0=t0: e.dma_start(out=o0[:], in_=YS[0][:, t0:t0 + 128, :].rearrange("h t i -> t h i")), writes=['o0'])
        dma(lambda e, t0=t0: e.dma_start(out=o1[:], in_=YS[1][:, t0:t0 + 128, :].rearrange("h t i -> t h i")), writes=['o1'])
        for (T, nm, src) in ((tr, 'otr', TR), (tv, 'otv', TV), (tg, 'otg', TG), (k0, 'ok0', TKD[0]), (k1, 'ok1', TKD[1])):
            dma(lambda e, T=T, src=src, t0=t0: e.dma_start(out=T[:], in_=src[t0:t0 + 128, :]), writes=[nm])
        op('dve', lambda e: e.tensor_tensor(out=o0[:], in0=o0[:], in1=o1[:], op=ALU.add), reads=['o0', 'o1'], writes=['o0'])
        op('dve', lambda e: e.reduce_sum(mean[:], o0[:], axis=AX.X), reads=['o0'], writes=['omean'])
        op('dve', lambda e: e.tensor_scalar(mean[:], mean[:], 1.0 / 64, None, op0=ALU.mult), reads=['omean'], writes=['omean'])
        op('dve', lambda e: e.tensor_tensor(out=o0[:], in0=o0[:], in1=mean[:].unsqueeze(2).to_broadcast([128, 8, 64]), op=ALU.subtract), reads=['o0', 'omean'], writes=['o0'])
        op('pool', lambda e: e.tensor_tensor(out=sq[:], in0=o0[:], in1=o0[:], op=ALU.mult), reads=['o0'], writes=['osq'])
        op('dve', lambda e: e.reduce_sum(var[:], sq[:], axis=AX.X), reads=['osq'], writes=['ovar'])
        op('dve', lambda e: e.tensor_scalar(var[:], var[:], 1.0 / 64, 64e-5, op0=ALU.mult, op1=ALU.add), reads=['ovar'], writes=['ovar'])
        op('act', lambda e: e.activation(var[:], var[:], AF.Sqrt), reads=['ovar'], writes=['ovar'])
        op('dve', lambda e: e.reciprocal(var[:], var[:]), reads=['ovar'], writes=['ovar'])
        op('dve', lambda e: e.tensor_tensor(out=o0[:], in0=o0[:], in1=var[:].unsqueeze(2).to_broadcast([128, 8, 64]), op=ALU.mult), reads=['o0', 'ovar'], writes=['o0'])
        op('dve', lambda e: e.tensor_tensor(out=o0[:], in0=o0[:], in1=v3(lng), op=ALU.mult), reads=['o0', 'lng'], writes=['o0'])
        op('pool', lambda e: e.tensor_tensor(out=o0[:], in0=o0[:], in1=v3(lnb), op=ALU.add), reads=['o0', 'lnb'], writes=['o0'])
        op('pool', lambda e: e.tensor_tensor(out=k0[:], in0=k0[:], in1=k1[:], op=ALU.add), reads=['ok0', 'ok1'], writes=['ok0'])
        op('pool', lambda e: e.tensor_tensor(out=k0[:], in0=k0[:], in1=rkb[:], op=ALU.mult), reads=['ok0', 'rkb'], writes=['ok0'])
        op('dve', lambda e: e.tensor_tensor(out=k0[:], in0=k0[:], in1=tr[:], op=ALU.mult), reads=['ok0', 'otr'], writes=['ok0'])
        op('dve', lambda e: e.reduce_sum(bon[:], v3(k0), axis=AX.X), reads=['ok0'], writes=['obon'])
        op('dve', lambda e: e.tensor_tensor(out=v3(tv), in0=v3(tv), in1=bon[:].unsqueeze(2).to_broadcast([128, 8, 64]), op=ALU.mult), reads=['otv', 'obon'], writes=['otv'])
        op('dve', lambda e: e.tensor_tensor(out=o0[:], in0=o0[:], in1=v3(tv), op=ALU.add), reads=['o0', 'otv'], writes=['o0'])
        op('dve', lambda e: e.tensor_tensor(out=o0[:], in0=o0[:], in1=v3(tg), op=ALU.mult), reads=['o0', 'otg'], writes=['o0'])
        for fb in range(4):
            P_ = pT[ct % 2]; pk_ = 'pTD%d' % (ct % 2); O = oT[ct % 2]; ok_ = 'oT%d' % (ct % 2); ct += 1
            op('pe', lambda e, P_=P_, fb=fb: e.transpose(P_[:], o0[:, 2 * fb:2 * fb + 2, :].rearrange("p a i -> p (a i)"), ident[:]), reads=['o0'], writes=[pk_])
            op('act', lambda e, P_=P_, O=O: e.copy(O[:], P_[:]), reads=[pk_], writes=[ok_])
            dma(lambda e, O=O, fb=fb, t0=t0: e.dma_start(out=Yd[fb * 128:(fb + 1) * 128, t0:t0 + 128], in_=O[:]), reads=[ok_])
    kb.end()


def _consts():
    c = {}
    c["c_ident"] = np.eye(128, dtype=np.float32)
    c["c_ones"] = np.ones((128, 128), np.float32)
    ps = np.zeros((128, 128), np.float32)
    for i in range(64):
        ps[2 * i, 2 * i + 1] = 1.0
        ps[2 * i + 1, 2 * i] = 1.0
    c["c_pswap"] = ps
    bo = np.zeros((128, 128), np.float32)
    bo[:64, :64] = 1.0
    bo[64:, 64:] = 1.0
    c["c_bones"] = bo
    sel = np.zeros((2, 256), np.float32)
    sel[0, 0:128] = 1.0
    sel[1, 128:256] = 1.0
    c["c_sel"] = sel
    t = np.arange(SEQ)
    r_idx = (t // 64).astype(np.float32)
    c_idx = (t % 64).astype(np.float32)

    def tables(rot_dim):
        n = rot_dim // 4
        inv = (10000.0 ** (-np.arange(n, dtype=np.float32) / n)).astype(np.float32)
        ang = np.concatenate([r_idx[:, None] * inv, c_idx[:, None] * inv], axis=-1).astype(np.float32)
        cos = np.cos(ang).astype(np.float32)
        sin = np.sin(ang).astype(np.float32)
        cosT = np.ones((rot_dim, TOK), np.float32)
        sinT = np.zeros((rot_dim, TOK), np.float32)
        for i in range(rot_dim // 2):
            cosT[2 * i, CTX:] = cos[:, i]
            cosT[2 * i + 1, CTX:] = cos[:, i]
            sinT[2 * i, CTX:] = -sin[:, i]
            sinT[2 * i + 1, CTX:] = sin[:, i]
        return cosT, sinT
    ca, sa = tables(32)
    cA = np.ones((128, TOK), np.float32); sA = np.zeros((128, TOK), np.float32)
    cA[64:96] = ca; sA[64:96] = sa
    c["c_cosA"] = cA; c["c_sinA"] = sA
    ch, sh = tables(64)
    c["c_cosH"] = np.concatenate([ch, ch], 0); c["c_sinH"] = np.concatenate([sh, sh], 0)
    k = np.arange(128)[:, None]; q = np.arange(128)[None, :]
    band = np.zeros((128, 256), np.float32)
    band[:, 0:128] = (q <= k)
    band[:, 128:256] = (k <= q)
    c["c_band"] = band
    c["c_iota16"] = np.tile(np.arange(16, dtype=np.float32)[None, :], (128, 1))
    hm = np.zeros((128, 512), np.float32)
    for r in range(128):
        hm[r, (r % 8) * 64:(r % 8) * 64 + 64] = 1.0
    c["c_hmask"] = hm
    return c


_CACHE = {}


def make_in_maps(inputs):
    consts = _consts()
    f = lambda a: np.ascontiguousarray(np.asarray(a, dtype=np.float32))
    shared = {k: f(inputs[k]) for k in ("ada_w", "ada_b", "w_in", "mla_q_norm", "mla_kv_norm", "mla_w_uq", "mla_w_ukv",
                                        "gqa_q_norm", "gqa_k_norm", "win_sink", "w_branch", "w_out", "ln1_g", "ln1_b", "ln2_g", "ln2_b",
                                        "peer_wq", "peer_keys", "peer_u", "peer_v", "rwkv_mu", "rwkv_w0", "rwkv_w2", "rwkv_a0", "rwkv_a2",
                                        "rwkv_g2", "rwkv_k_k", "rwkv_k_a", "rwkv_ln_g", "rwkv_ln_b")}
    shared["rwkv_r_k"] = f(inputs["rwkv_r_k"]).reshape(DEPTH, 512)
    shared.update(consts)
    maps = []
    for b in range(8):
        m = dict(shared)
        m["xin"] = np.ascontiguousarray(np.concatenate([f(inputs["ctx"][b]), f(inputs["x"][b])], axis=0))
        m["cc"] = np.ascontiguousarray(np.stack([f(inputs["c"][b]), f(inputs["c_ctx"])], axis=0))
        maps.append(m)
    return maps


def kernel(**inputs):
    kb = build()
    maps = make_in_maps(inputs)
    res = run_bass_kernel_spmd(kb.nc, maps, core_ids=list(range(8)))
    return np.stack([np.asarray(r["yout"], dtype=np.float32) for r in res.results], axis=0)
```
